# Optimizing a Trainium2 kernel written in Bass

```python
import math
import jax, jax.numpy as jnp
from jax import lax
import numpy as np

D_MODEL = 2048
BATCH = 8
SEQ = 4096
DEPTH = 2

CHUNK = 64
Q_BLOCK = 128
N_MIXERS = 2
N_CONV_LAYERS = (DEPTH + 1) // 2
N_ATTN_LAYERS = DEPTH // 2
N_DENSE_FFN_LAYERS = (DEPTH + 1) // 2
N_MOE_LAYERS = DEPTH // 2
CONV_WIDTH = 31
DIFF_HEADS = 16
DIFF_HEAD_DIM = D_MODEL // (2 * DIFF_HEADS)
D_FF_DENSE = ((8 * D_MODEL // 3 + 255) // 256) * 256
N_EXPERTS = 8
TOP_K = 2
D_FF_EXPERT = 7 * D_MODEL // 2
RMS_EPS = 1e-6
LN_EPS = 1e-5

kernel_name = "hybrid_conformer_diffattn_moe_adaln"


def rms_norm(x, g):
    xf = x.astype(jnp.float32)
    y = xf * lax.rsqrt(jnp.mean(xf * xf, axis=-1, keepdims=True) + RMS_EPS)
    return (y * g.astype(jnp.float32)).astype(x.dtype)


def layer_norm(x, g, b):
    xf = x.astype(jnp.float32)
    mu = jnp.mean(xf, axis=-1, keepdims=True)
    var = jnp.mean(jnp.square(xf - mu), axis=-1, keepdims=True)
    y = (xf - mu) * lax.rsqrt(var + LN_EPS)
    return (y * g.astype(jnp.float32) + b.astype(jnp.float32)).astype(x.dtype)


def modulate(h, shift, scale):
    return h * (1.0 + scale[:, None, :]) + shift[:, None, :]


def alibi_slopes(n_heads):
    ratio = 2.0 ** (-8.0 / n_heads)
    return jnp.asarray(np.array([ratio ** (h + 1) for h in range(n_heads)], dtype=np.float32))


def diff_lambda_init(layer_idx):
    return 0.8 - 0.6 * math.exp(-0.3 * layer_idx)


def conformer_conv(h, w_in, b_in, w_dw, b_dw, ln_g, ln_b, w_out, b_out):
    u = h @ w_in + b_in
    val, gate = jnp.split(u, 2, axis=-1)
    u = val * jax.nn.sigmoid(gate)
    u = lax.conv_general_dilated(
        u, w_dw[:, None, :].astype(u.dtype),
        window_strides=(1,), padding=((CONV_WIDTH - 1, 0),),
        dimension_numbers=("NWC", "WIO", "NWC"),
        feature_group_count=D_MODEL) + b_dw
    u = jax.nn.silu(layer_norm(u, ln_g, ln_b))
    return u @ w_out + b_out


def diff_attention(h, w_qkv, w_o, lam_q1, lam_k1, lam_q2, lam_k2, subln_g, lambda_init):
    B, S, _ = h.shape
    H, dh = DIFF_HEADS, DIFF_HEAD_DIM
    q, k, v = jnp.split(h @ w_qkv, 3, axis=-1)
    q = q.reshape(B, S, H, 2, dh).astype(jnp.float32) * (dh ** -0.5)
    k = k.reshape(B, S, H, 2, dh).astype(jnp.float32)
    v = v.reshape(B, S, H, 2 * dh)
    lam = (jnp.exp(jnp.sum(lam_q1.astype(jnp.float32) * lam_k1.astype(jnp.float32)))
           - jnp.exp(jnp.sum(lam_q2.astype(jnp.float32) * lam_k2.astype(jnp.float32)))
           + lambda_init)
    slopes = alibi_slopes(H)
    pos = jnp.arange(S, dtype=jnp.int32)
    chunk_id = pos // CHUNK
    outs = []
    for qb in range(S // Q_BLOCK):
        q0 = qb * Q_BLOCK
        kend = q0 + Q_BLOCK
        s = jnp.einsum("bqhmd,bkhmd->bhmqk", q[:, q0:kend], k[:, :kend])
        dist = jnp.abs(pos[q0:kend, None] - pos[None, :kend]).astype(jnp.float32)
        bias = -slopes[:, None, None] * dist[None]
        allowed = chunk_id[None, :kend] <= chunk_id[q0:kend, None]
        s = jnp.where(allowed, s + bias[None, :, None], -jnp.inf)
        p = jax.nn.softmax(s, axis=-1)
        attn = p[:, :, 0] - lam * p[:, :, 1]
        outs.append(jnp.einsum("bhqk,bkhe->bqhe", attn.astype(v.dtype), v[:, :kend]))
    o = jnp.concatenate(outs, axis=1)
    o = rms_norm(o, subln_g) * (1.0 - lambda_init)
    return o.reshape(B, S, H * 2 * dh) @ w_o


def swiglu(h, w_gate, w_up, w_down):
    return (jax.nn.silu(h @ w_gate) * (h @ w_up)) @ w_down


def moe_swiglu(h, w_router, w_gate, w_up, w_down):
    B, S, D = h.shape
    t = h.reshape(B * S, D)
    logits = (t @ w_router).astype(jnp.float32)
    top_v, top_i = lax.top_k(logits, TOP_K)
    top_w = jax.nn.softmax(top_v, axis=-1)
    combine = jnp.sum(jax.nn.one_hot(top_i, N_EXPERTS, dtype=jnp.float32) * top_w[..., None], axis=1)
    y = jnp.zeros_like(t)
    for e in range(N_EXPERTS):
        y = y + combine[:, e:e + 1].astype(t.dtype) * swiglu(t, w_gate[e], w_up[e], w_down[e])
    return y.reshape(B, S, D)


def setup_inputs(seed: int = 0) -> dict:
    key = jax.random.key(seed)
    ks = list(jax.random.split(key, 32))
    it = iter(ks)
    D = D_MODEL

    def nrm(shape, scale):
        return scale * jax.random.normal(next(it), shape, jnp.float32)

    nA, nB, nD, nE = N_CONV_LAYERS, N_ATTN_LAYERS, N_DENSE_FFN_LAYERS, N_MOE_LAYERS
    return {
        "x": nrm((BATCH, SEQ, D), 1.0),
        "c": nrm((BATCH, D), 1.0),
        "mod_w": nrm((DEPTH, D, 6 * D), 0.3 * D ** -0.5),
        "mod_b": nrm((DEPTH, 6 * D), 0.02),
        "norm1_g": 1.0 + nrm((DEPTH, D), 0.02),
        "norm2_g": 1.0 + nrm((DEPTH, D), 0.02),
        "conv_w_in": nrm((nA, D, 2 * D), D ** -0.5),
        "conv_b_in": nrm((nA, 2 * D), 0.02),
        "conv_w_dw": nrm((nA, CONV_WIDTH, D), CONV_WIDTH ** -0.5),
        "conv_b_dw": nrm((nA, D), 0.02),
        "conv_ln_g": 1.0 + nrm((nA, D), 0.02),
        "conv_ln_b": nrm((nA, D), 0.02),
        "conv_w_out": nrm((nA, D, D), D ** -0.5),
        "conv_b_out": nrm((nA, D), 0.02),
        "attn_w_qkv": nrm((nB, D, 3 * D), D ** -0.5),
        "attn_w_o": nrm((nB, D, D), D ** -0.5),
        "attn_lam_q1": nrm((nB, DIFF_HEAD_DIM), 0.1),
        "attn_lam_k1": nrm((nB, DIFF_HEAD_DIM), 0.1),
        "attn_lam_q2": nrm((nB, DIFF_HEAD_DIM), 0.1),
        "attn_lam_k2": nrm((nB, DIFF_HEAD_DIM), 0.1),
        "attn_subln_g": 1.0 + nrm((nB, 2 * DIFF_HEAD_DIM), 0.02),
        "ffn_w_gate": nrm((nD, D, D_FF_DENSE), D ** -0.5),
        "ffn_w_up": nrm((nD, D, D_FF_DENSE), D ** -0.5),
        "ffn_w_down": nrm((nD, D_FF_DENSE, D), D_FF_DENSE ** -0.5),
        "moe_w_router": nrm((nE, D, N_EXPERTS), D ** -0.5),
        "moe_w_gate": nrm((nE, N_EXPERTS, D, D_FF_EXPERT), D ** -0.5),
        "moe_w_up": nrm((nE, N_EXPERTS, D, D_FF_EXPERT), D ** -0.5),
        "moe_w_down": nrm((nE, N_EXPERTS, D_FF_EXPERT, D), D_FF_EXPERT ** -0.5),
        "final_g": 1.0 + nrm((D,), 0.02),
    }


def reference(x, c, mod_w, mod_b, norm1_g, norm2_g,
              conv_w_in, conv_b_in, conv_w_dw, conv_b_dw, conv_ln_g, conv_ln_b, conv_w_out, conv_b_out,
              attn_w_qkv, attn_w_o, attn_lam_q1, attn_lam_k1, attn_lam_q2, attn_lam_k2, attn_subln_g,
              ffn_w_gate, ffn_w_up, ffn_w_down,
              moe_w_router, moe_w_gate, moe_w_up, moe_w_down,
              final_g):
    c_act = jax.nn.silu(c)
    for i in range(DEPTH):
        j = i // N_MIXERS
        mod = c_act @ mod_w[i] + mod_b[i]
        sh1, sc1, g1, sh2, sc2, g2 = jnp.split(mod, 6, axis=-1)
        h = modulate(rms_norm(x, norm1_g[i]), sh1, sc1)
        if i % N_MIXERS == 0:
            mix = conformer_conv(h, conv_w_in[j], conv_b_in[j], conv_w_dw[j], conv_b_dw[j],
                                 conv_ln_g[j], conv_ln_b[j], conv_w_out[j], conv_b_out[j])
        else:
            mix = diff_attention(h, attn_w_qkv[j], attn_w_o[j], attn_lam_q1[j], attn_lam_k1[j],
                                 attn_lam_q2[j], attn_lam_k2[j], attn_subln_g[j], diff_lambda_init(i))
        x = x + g1[:, None, :] * mix
        h = modulate(rms_norm(x, norm2_g[i]), sh2, sc2)
        if i % 2 == 0:
            ffn = swiglu(h, ffn_w_gate[j], ffn_w_up[j], ffn_w_down[j])
        else:
            ffn = moe_swiglu(h, moe_w_router[j], moe_w_gate[j], moe_w_up[j], moe_w_down[j])
        x = x + g2[:, None, :] * ffn
    return rms_norm(x, final_g)
```

```python
import math
import numpy as np
import concourse.bass as bass
import concourse.mybir as mybir
from concourse.bass_utils import run_bass_kernel_spmd

F32 = mybir.dt.float32
BF16 = mybir.dt.bfloat16
AF = mybir.ActivationFunctionType
ALU = mybir.AluOpType
AX = mybir.AxisListType

D = 2048
S = 4096
B = 8
KC = 16
TT = 512
NT = S // TT
FF = 5632
FFC = FF // 128
EF = 7168
EFC = EF // 128
NE = 8
H = 16
CW = 31
HALO = CW - 1
RMS_EPS = 1e-6
LN_EPS = 1e-5
NEG = -30000.0

V_N1G = 0
V_N2G = 32
V_MODB = 64
V_CBIN = 256
V_CWDW = 288
V_CBDW = 784
V_LNG = 800
V_LNB = 816
V_CBOUT = 832
V_FING = 848
V_SUBG = 864
V_LAM = 865
V_TOT = 1121
C_ID = 0
C_ABSD = 128
C_DMASK = 256
C_SEL = 384
C_TOT = 384 + 1024


def lam_init(layer_idx):
    return 0.8 - 0.6 * math.exp(-0.3 * layer_idx)


class Buf:
    __slots__ = ("name", "w", "r")

    def __init__(self, name):
        self.name = name
        self.w = {}
        self.r = {}


class Eng:
    def __init__(self, name, obj, sem, is_pe=False):
        self.name = name
        self.obj = obj
        self.sem = sem
        self.is_pe = is_pe
        self.count = 0
        self.pending = False
        self.waited = {}
        self.dma_sems = []
        self.dma_uses = []
        self.dma_next = 0


class Tracker:
    def __init__(self, nc, n_dma_sems=10):
        self.nc = nc
        self.sems = []

        def newsem(name):
            s = nc.alloc_semaphore(name)
            self.sems.append(s)
            return len(self.sems) - 1

        self.eng = {
            "pe": Eng("pe", nc.tensor, newsem("s_pe"), is_pe=True),
            "act": Eng("act", nc.scalar, newsem("s_act")),
            "dve": Eng("dve", nc.vector, newsem("s_dve")),
            "pool": Eng("pool", nc.gpsimd, newsem("s_pool")),
            "sp": Eng("sp", nc.sync, newsem("s_sp")),
        }
        for q in ("sp", "pool", "act"):
            e = self.eng[q]
            for i in range(n_dma_sems):
                e.dma_sems.append(newsem(f"d_{q}{i}"))
                e.dma_uses.append(0)

    def _wait(self, e, need):
        for s, v in need.items():
            if e.waited.get(s, 0) >= v:
                continue
            e.obj.wait_ge(self.sems[s], v)
            e.waited[s] = v

    def _need(self, e, r, w, nowaw=False):
        need = {}

        def add(d, allow_same):
            for s, v in d.items():
                if s == e.sem and not allow_same:
                    continue
                if need.get(s, 0) < v:
                    need[s] = v

        for b in r:
            add(b.w, not e.is_pe)
        for b in w:
            if not nowaw:
                add(b.w, not e.is_pe)
            add(b.r, False)
        return need

    def op(self, en, fn, r=(), w=(), inc=True):
        e = self.eng[en]
        need = self._need(e, r, w)
        if e.pending:
            assert need.get(e.sem, 0) <= e.count, "self-dep on pending token"
        self._wait(e, need)
        ins = fn()
        tok = e.count + 1
        if inc:
            ins.then_inc(self.sems[e.sem], 1)
            e.count = tok
            e.pending = False
        else:
            assert e.is_pe
            e.pending = True
        for b in r:
            if b.r.get(e.sem, 0) < tok:
                b.r[e.sem] = tok
        for b in w:
            b.w = {e.sem: tok}
            b.r = {}
        return ins

    def dma(self, q, out, in_, r=(), w=(), accum=False, **kw):
        e = self.eng[q]
        k = e.dma_next
        e.dma_next = (k + 1) % len(e.dma_sems)
        s = e.dma_sems[k]
        uses = e.dma_uses[k]
        need = self._need(e, r, w, nowaw=accum)
        if uses > 0 and need.get(s, 0) < 16 * uses:
            need[s] = 16 * uses
        self._wait(e, need)
        ins = e.obj.dma_start(out=out, in_=in_, **kw)
        ins.then_inc(self.sems[s], 16)
        e.dma_uses[k] = uses + 1
        tok = 16 * (uses + 1)
        for b in r:
            if b.r.get(s, 0) < tok:
                b.r[s] = tok
        for b in w:
            if accum:
                if b.w.get(s, 0) < tok:
                    b.w[s] = tok
            else:
                b.w = {s: tok}
                b.r = {}
        return ins

    def barrier(self):
        need = {}
        for e in self.eng.values():
            assert not e.pending
            if e.count > 0:
                need[e.sem] = e.count
            for s, u in zip(e.dma_sems, e.dma_uses):
                if u > 0:
                    need[s] = 16 * u
        for e in self.eng.values():
            n2 = {s: v for s, v in need.items() if s != e.sem}
            self._wait(e, n2)


class Tile:
    def __init__(self, t, name):
        self.t = t
        self.b = Buf(name)

    def __getitem__(self, idx):
        return self.t[idx]


def build(stage=99, dbg=None, dbg_tile=None, ne_run=NE, mode="full", e0=0, first=True, last=True):
    nc = bass.Bass("TRN2", target_bir_lowering=False)
    T = Tracker(nc)

    def din(name, shape, dt=F32):
        return nc.dram_tensor(name, list(shape), dt, kind="ExternalInput").ap()

    def dscr(name, shape, dt):
        return Tile(nc.dram_tensor(name, list(shape), dt, kind="Internal").ap(), name)

    front = mode in ("full", "front")
    vecs_in = din("vecs", [128, V_TOT])
    consts_in = din("consts", [128, C_TOT])
    wr_in = din("wr", [D, NE])
    w_in = {}
    if front:
        xT_in = din("xT", [KC, 128, S])
        cT_in = din("cT", [128, KC])
        base_in = din("base", [128, S])
        mod_w = din("mod_w", [2, D, 6 * D])
        w_in = {
            "cwin": din("cwin", [D, 2 * D]),
            "cwout": din("cwout", [D, D]),
            "fwg": din("fwg", [D, FF]),
            "fwu": din("fwu", [D, FF]),
            "fwd": din("fwd", [FF, D]),
            "wqkv": din("wqkv", [D, 3 * D]),
            "wo": din("wo", [D, D]),
        }
    else:
        x2T_in = din("x2T", [KC, 128, S])
        mvec_in = din("mvec", [128, 368])
        if not first:
            xacc_in = din("xaccT", [KC, 128, S])
    if mode == "front":
        mvec_out = Tile(nc.dram_tensor("mvec_out", [128, 368], F32, kind="ExternalOutput").ap(), "mvec_out")
    if stage >= 3 and mode != "front":
        mwg_in = din("mwg", [ne_run, D, EF])
        mwu_in = din("mwu", [ne_run, D, EF])
        mwd_in = din("mwd", [ne_run, EF, D])
    outT = Tile(nc.dram_tensor("outT", [KC, 128, S], F32, kind="ExternalOutput").ap(), "outT")

    wb = {}
    for k, a in w_in.items():
        wb[k] = dscr("b_" + k, a.shape, BF16)
    mwg_b = [dscr(f"b_mwg{e}", [D, EF], BF16) for e in range(ne_run)]
    mwu_b = [dscr(f"b_mwu{e}", [D, EF], BF16) for e in range(ne_run)]
    mwd_b = [dscr(f"b_mwd{e}", [EF, D], BF16) for e in range(ne_run)]
    x1T = dscr("x1T", [KC, 128, S], F32)
    qT_s = dscr("qT_s", [H, 128, S], BF16)
    kT_s = dscr("kT_s", [H, 128, S], BF16)
    v_s = dscr("v_s", [H, 128, S // 128, 128], BF16)

    off = {"p": 16512}
    uid = {"n": 0}

    def _nbytes(shape, dt):
        n = 1
        for s_ in shape[1:]:
            n *= s_
        return n * (2 if dt == BF16 else 4)

    def sbp(name, shape, dt):
        t = nc.alloc_sbuf_tensor_at(name, list(shape), dt, offset=off["p"])
        off["p"] += (_nbytes(shape, dt) + 31) // 32 * 32
        return Tile(t, name)

    def ps_alloc(name):
        return Tile(nc.alloc_psum_tensor(name, [128, 512], F32), name)

    vecs = sbp("vecs", [128, V_TOT], F32)
    consts = sbp("consts", [128, C_TOT], F32)
    modT = sbp("modT", [128, 2 * 96], F32)
    drv = sbp("drv", [128, 176], F32)
    cvec = sbp("cvec", [128, 64], F32)
    ones_bf = sbp("ones_bf", [128, 128], BF16)
    wr_sb = sbp("wr_sb", [128, KC, NE], F32)
    xs = sbp("xs", [128, KC, TT], F32)
    hT = sbp("hT", [128, KC, TT], BF16)
    NSLAB = 4
    slabs = [sbp(f"slab{i}", [128, KC, 512], BF16) for i in range(NSLAB)]
    NTMP = 5
    tmps = [sbp(f"tmp{i}", [128, 512], F32) for i in range(NTMP)]
    NLT = 3
    ltmps = [sbp(f"ltmp{i}", [128, 512], F32) for i in range(NLT)]
    R0 = off["p"]
    RCAP = 229376 - R0
    assert RCAP >= 66496, RCAP

    def sbr(name, shape, dt, o, buf=None):
        assert o + _nbytes(shape, dt) <= RCAP, (name, o, _nbytes(shape, dt), RCAP)
        uid["n"] += 1
        t = nc.alloc_sbuf_tensor_at(f"{name}_{uid['n']}", list(shape), dt, offset=R0 + o)
        tl = Tile(t, name)
        if buf is not None:
            tl.b = buf
        return tl

    psb = [ps_alloc(f"ps{i}") for i in range(8)]
    st = {"slab": 0, "tmp": 0, "ps": 0, "lt": 0}

    def next_slab():
        s = slabs[st["slab"] % NSLAB]
        st["slab"] += 1
        return s

    def tmp():
        s = tmps[st["tmp"] % NTMP]
        st["tmp"] += 1
        return s

    def ltmp():
        s = ltmps[st["lt"] % NLT]
        st["lt"] += 1
        return s

    def ps(lo=0, hi=8):
        n = hi - lo
        s = psb[lo + st["ps"] % n]
        st["ps"] += 1
        return s

    def V(col, n=1):
        return vecs[:, col:col + n]

    def mm(out_ap, lhsT, rhs, start, stop, r, w, inc):
        return T.op("pe", lambda: nc.tensor.matmul(out_ap, lhsT=lhsT, rhs=rhs, start=start, stop=stop),
                    r=r, w=w, inc=inc)

    def load_slab(wt, kc0, nkc, c0, ncols=512):
        s = next_slab()
        src = wt.t.rearrange("(kc p) n -> p kc n", p=128)[:, kc0:kc0 + nkc, c0:c0 + ncols]
        T.dma("sp", s[:, 0:nkc, 0:ncols], src, r=[wt.b], w=[s.b])
        return s

    def xview(tl):
        return tl.rearrange("c p t -> p c t")

    def precast(src_ap, dst, K, N):
        RBK = 128
        for i in range(K // RBK):
            o = dst.t[i * RBK:(i + 1) * RBK, :].rearrange("k (a b) -> k a b", b=512)
            ii = src_ap[i * RBK:(i + 1) * RBK, :].rearrange("k (a b) -> k a b", b=512)
            T.dma("pool", o, ii, w=[dst.b], accum=True)

    T.dma("sp", vecs[:, :], vecs_in[:, :], w=[vecs.b])
    T.dma("sp", consts[:, :], consts_in[:, :], w=[consts.b])
    if front:
        T.dma("sp", cvec[:, 0:KC], cT_in[:, :], w=[cvec.b])
    else:
        T.dma("sp", modT[:, :], mvec_in[:, 0:192], w=[modT.b])
        T.dma("sp", drv[:, :], mvec_in[:, 192:368], w=[drv.b])
    T.dma("sp", wr_sb[:, :, :], wr_in.rearrange("(kc p) e -> p kc e", p=128), w=[wr_sb.b])
    T.op("dve", lambda: nc.vector.memset(ones_bf[:, :], 1.0), w=[ones_bf.b])

    for k in ("cwin", "cwout", "fwg", "fwu", "fwd", "wqkv", "wo"):
        if front and (stage >= 2 or k in ("cwin", "cwout", "fwg", "fwu", "fwd")):
            precast(w_in[k], wb[k], w_in[k].shape[0], w_in[k].shape[1])
    if stage >= 3 and mode != "front":
        for e in range(ne_run):
            precast(mwg_in[e], mwg_b[e], D, EF)
            precast(mwu_in[e], mwu_b[e], D, EF)
            precast(mwd_in[e], mwd_b[e], EF, D)

    def phase0():
        T.op("act", lambda: nc.scalar.activation(out=cvec[:, 16:32], in_=cvec[:, 0:16], func=AF.Silu),
             r=[cvec.b], w=[cvec.b])
        MSL = 512
        mws = [sbr("mw0", [128, KC, MSL], F32, 0), sbr("mw1", [128, KC, MSL], F32, 32768)]
        cnt = 0
        for l in range(2):
            pm = ps()
            for sgi in range(6 * D // MSL):
                mwt = mws[cnt % 2]
                cnt += 1
                src = mod_w[l].rearrange("(kc p) n -> p kc n", p=128)[:, :, sgi * MSL:(sgi + 1) * MSL]
                T.dma("sp", mwt[:, :, :], src, w=[mwt.b])
                for j4 in range(MSL // 128):
                    j = sgi * (MSL // 128) + j4
                    for kc in range(KC):
                        mm(pm[:, j:j + 1], mwt[:, kc, j4 * 128:(j4 + 1) * 128], cvec[:, 16 + kc:17 + kc],
                           kc == 0, kc == KC - 1, r=[mwt.b, cvec.b], w=[pm.b],
                           inc=(kc == KC - 1 and j4 == MSL // 128 - 1))
            T.op("dve", lambda: nc.vector.tensor_tensor(out=modT[:, l * 96:(l + 1) * 96], in0=pm[:, 0:96],
                                                        in1=V(V_MODB + l * 96, 96), op=ALU.add),
                 r=[pm.b, vecs.b], w=[modT.b])
        for l in range(2):
            o = l * 80
            m = l * 96
            T.op("dve", lambda: nc.vector.scalar_tensor_tensor(
                out=drv[:, o:o + 16], in0=modT[:, m + 16:m + 32], scalar=1.0, in1=V(V_N1G + l * 16, 16),
                op0=ALU.add, op1=ALU.mult), r=[modT.b, vecs.b], w=[drv.b])
            T.op("dve", lambda: nc.vector.scalar_tensor_tensor(
                out=drv[:, o + 16:o + 32], in0=modT[:, m + 64:m + 80], scalar=1.0, in1=V(V_N2G + l * 16, 16),
                op0=ALU.add, op1=ALU.mult), r=[modT.b, vecs.b], w=[drv.b])
        T.op("dve", lambda: nc.vector.tensor_tensor(out=drv[:, 32:48], in0=modT[:, 32:48], in1=V(V_CBOUT, 16),
                                                    op=ALU.mult), r=[modT.b, vecs.b], w=[drv.b])
        t0 = tmp()
        T.op("dve", lambda: nc.vector.tensor_tensor(out=t0[:, 0:64], in0=V(V_LAM, 64), in1=V(V_LAM + 64, 64),
                                                    op=ALU.mult), r=[vecs.b], w=[t0.b])
        T.op("dve", lambda: nc.vector.tensor_tensor(out=t0[:, 64:128], in0=V(V_LAM + 128, 64), in1=V(V_LAM + 192, 64),
                                                    op=ALU.mult), r=[vecs.b], w=[t0.b])
        T.op("dve", lambda: nc.vector.reduce_sum(out=drv[:, 162:163], in_=t0[:, 0:64], axis=AX.X), r=[t0.b], w=[drv.b])
        T.op("dve", lambda: nc.vector.reduce_sum(out=drv[:, 163:164], in_=t0[:, 64:128], axis=AX.X), r=[t0.b], w=[drv.b])
        T.op("act", lambda: nc.scalar.activation(out=drv[:, 164:166], in_=drv[:, 162:164], func=AF.Exp),
             r=[drv.b], w=[drv.b])
        T.op("dve", lambda: nc.vector.scalar_tensor_tensor(
            out=drv[:, 160:161], in0=drv[:, 164:165], scalar=lam_init(1), in1=drv[:, 165:166],
            op0=ALU.add, op1=ALU.subtract), r=[drv.b], w=[drv.b])
        T.op("dve", lambda: nc.vector.tensor_scalar(out=drv[:, 161:162], in0=drv[:, 160:161], scalar1=-1.0, scalar2=None,
                                                    op0=ALU.mult), r=[drv.b], w=[drv.b])
        T.op("dve", lambda: nc.vector.tensor_scalar(out=drv[:, 166:167], in0=V(V_SUBG, 1), scalar1=1.0 - lam_init(1),
                                                    scalar2=None, op0=ALU.mult), r=[vecs.b], w=[drv.b])
        T.barrier()


    if front:
        phase0()
    else:
        T.barrier()

    def MOD(l, which):
        m = l * 96 + which * 16
        return lambda c: modT[:, m + c:m + c + 1]

    def DRV(col):
        return lambda c: drv[:, col + c:col + c + 1]

    cur = {}

    def rms_stats(src3):
        sq = cur["sq"]
        T.op("act", lambda: nc.scalar.activation(out=sq[:, :, :], in_=src3[:, :, :], func=AF.Square),
             r=[src3.b], w=[sq.b])
        p = ps()
        for kc in range(KC):
            mm(p[:, :], ones_bf[:, :], sq[:, kc, :], kc == 0, kc == KC - 1, r=[sq.b, ones_bf.b], w=[p.b],
               inc=(kc == KC - 1))
        rstd = ltmp()
        T.op("act", lambda: nc.scalar.activation(out=rstd[:, :], in_=p[:, :], func=AF.Sqrt, bias=RMS_EPS,
                                                 scale=1.0 / D), r=[p.b], w=[rstd.b])
        T.op("dve", lambda: nc.vector.reciprocal(out=rstd[:, :], in_=rstd[:, :]), r=[rstd.b], w=[rstd.b])
        return rstd

    def rms_mod(A, Bsh, post_chunk=None):
        rstd = rms_stats(xs)
        for c in range(KC):
            t = tmp()
            T.op("dve", lambda: nc.vector.scalar_tensor_tensor(
                out=t[:, :], in0=xs[:, c, :], scalar=A(c), in1=rstd[:, :], op0=ALU.mult, op1=ALU.mult),
                r=[xs.b, rstd.b, drv.b, modT.b], w=[t.b])
            if post_chunk is None:
                T.op("act", lambda: nc.scalar.activation(out=hT[:, c, :], in_=t[:, :], func=AF.Identity,
                                                         bias=Bsh(c), scale=1.0),
                     r=[t.b, modT.b], w=[hT.b])
            else:
                T.op("dve", lambda: nc.vector.tensor_scalar(out=t[:, :], in0=t[:, :], scalar1=Bsh(c), scalar2=None,
                                                            op0=ALU.add), r=[t.b, modT.b], w=[t.b])
                T.op("act", lambda: nc.scalar.activation(out=hT[:, c, :], in_=t[:, :], func=AF.Copy),
                     r=[t.b], w=[hT.b])
                post_chunk(c, t)

    def down_proj(wt, nkc, actb, G, kgroup=KC):
        kgs = []
        k0 = 0
        while k0 < nkc:
            kgs.append((k0, min(kgroup, nkc - k0)))
            k0 += kgroup
        for cg in range(4):
            pbank = [ps(0, 4) for _ in range(4)]
            for gi, (k0, nk) in enumerate(kgs):
                s = load_slab(wt, k0, nk, cg * 512)
                for j4 in range(4):
                    p = pbank[j4]
                    for kk in range(nk):
                        first = (gi == 0 and kk == 0)
                        last = (gi == len(kgs) - 1 and kk == nk - 1)
                        mm(p[:, :], s[:, kk, j4 * 128:(j4 + 1) * 128], actb[:, k0 + kk, :], first, last,
                           r=[s.b, actb.b], w=[p.b], inc=(kk == nk - 1))
            for j4 in range(4):
                mc = cg * 4 + j4
                p = pbank[j4]
                T.op("dve", lambda: nc.vector.scalar_tensor_tensor(
                    out=xs[:, mc, :], in0=p[:, :], scalar=G(mc), in1=xs[:, mc, :], op0=ALU.mult, op1=ALU.add),
                    r=[p.b, xs.b, modT.b], w=[xs.b])

    def gate_up(wg, wu, nslab, actb, cb=None):
        for sgi in range(nslab):
            s1 = load_slab(wg, 0, KC, sgi * 512)
            s2 = load_slab(wu, 0, KC, sgi * 512)
            for j4 in range(4):
                j = sgi * 4 + j4
                pg = ps(4, 8)
                pu = ps(4, 8)
                for kc in range(KC):
                    mm(pg[:, :], s1[:, kc, j4 * 128:(j4 + 1) * 128], hT[:, kc, :], kc == 0, kc == KC - 1,
                       r=[s1.b, hT.b], w=[pg.b], inc=(kc == KC - 1))
                for kc in range(KC):
                    mm(pu[:, :], s2[:, kc, j4 * 128:(j4 + 1) * 128], hT[:, kc, :], kc == 0, kc == KC - 1,
                       r=[s2.b, hT.b], w=[pu.b], inc=(kc == KC - 1))
                sg_ = tmp()
                T.op("act", lambda: nc.scalar.activation(out=sg_[:, :], in_=pg[:, :], func=AF.Silu),
                     r=[pg.b], w=[sg_.b])
                if cb is not None:
                    T.op("dve", lambda: nc.vector.tensor_tensor(out=sg_[:, :], in0=sg_[:, :], in1=cb[:, :],
                                                                op=ALU.mult), r=[sg_.b, cb.b], w=[sg_.b])
                T.op("dve", lambda: nc.vector.tensor_tensor(out=actb[:, j, :], in0=pu[:, :], in1=sg_[:, :],
                                                            op=ALU.mult), r=[pu.b, sg_.b], w=[actb.b])


    class DbgStop(Exception):
        pass

    def dump(src3, buf, n=KC, tile=0):
        T.op("act", lambda: nc.scalar.activation(out=xs[:, 0:n, :], in_=src3, func=AF.Copy), r=[buf], w=[xs.b])
        T.dma("sp", xview(outT.t)[:, :, tile * TT:(tile + 1) * TT], xs[:, :, :], r=[xs.b], w=[outT.b], accum=True)
        T.barrier()
        raise DbgStop()

    def phase_ab():
        ub = sbr("ub", [128, KC, HALO + TT], BF16, 0)
        UO = 17344
        ubuf = Buf("U")
        vb = sbr("vb", [128, KC, TT], F32, UO, ubuf)
        sq = sbr("sq", [128, KC, TT], BF16, UO + 32768, ubuf)
        actb = sbr("actb", [128, FFC, TT], BF16, UO, ubuf)
        cur["sq"] = sq
        hsave = sbr("hsave", [128, KC, HALO], F32, UO + 49152)
        T.op("dve", lambda: nc.vector.memset(ub[:, :, 0:HALO], 0.0), w=[ub.b])
        for i in range(NT):
            T.dma("sp", xs[:, :, :], xview(xT_in)[:, :, i * TT:(i + 1) * TT], w=[xs.b])
            rms_mod(DRV(0), MOD(0, 0))
            if dbg == 'mod':
                T.op('act', lambda: nc.scalar.activation(out=xs[:, 0, 0:192], in_=modT[:, :], func=AF.Copy), r=[modT.b], w=[xs.b])
                T.op('act', lambda: nc.scalar.activation(out=xs[:, 1, 0:176], in_=drv[:, :], func=AF.Copy), r=[drv.b], w=[xs.b])
                dump(xs[:, :, :], xs.b)
            if dbg == 'h1':
                dump(hT[:, :, :], hT.b)
            for sgi in range(4):
                sv = load_slab(wb["cwin"], 0, KC, sgi * 512)
                sg = load_slab(wb["cwin"], 0, KC, D + sgi * 512)
                for j4 in range(4):
                    j = sgi * 4 + j4
                    pv = ps()
                    pg = ps()
                    for kc in range(KC):
                        mm(pv[:, :], sv[:, kc, j4 * 128:(j4 + 1) * 128], hT[:, kc, :], kc == 0, kc == KC - 1,
                           r=[sv.b, hT.b], w=[pv.b], inc=(kc == KC - 1))
                    for kc in range(KC):
                        mm(pg[:, :], sg[:, kc, j4 * 128:(j4 + 1) * 128], hT[:, kc, :], kc == 0, kc == KC - 1,
                           r=[sg.b, hT.b], w=[pg.b], inc=(kc == KC - 1))
                    sgm = tmp()
                    T.op("act", lambda: nc.scalar.activation(out=sgm[:, :], in_=pg[:, :], func=AF.Sigmoid,
                                                             bias=V(V_CBIN + 16 + j), scale=1.0),
                         r=[pg.b, vecs.b], w=[sgm.b])
                    T.op("dve", lambda: nc.vector.scalar_tensor_tensor(
                        out=ub[:, j, HALO:HALO + TT], in0=pv[:, :], scalar=V(V_CBIN + j), in1=sgm[:, :],
                        op0=ALU.add, op1=ALU.mult), r=[pv.b, sgm.b, vecs.b], w=[ub.b])
            if dbg == 'u':
                dump(ub[:, :, HALO:HALO + TT], ub.b)
            for j in range(KC):
                for wtap in range(CW):
                    wcol = V(V_CWDW + j * CW + wtap)
                    if wtap == 0:
                        T.op("dve", lambda: nc.vector.tensor_scalar(
                            out=vb[:, j, :], in0=ub[:, j, 0:TT], scalar1=wcol, scalar2=V(V_CBDW + j),
                            op0=ALU.mult, op1=ALU.add), r=[ub.b, vecs.b], w=[vb.b])
                    else:
                        T.op("dve", lambda: nc.vector.scalar_tensor_tensor(
                            out=vb[:, j, :], in0=ub[:, j, wtap:wtap + TT], scalar=wcol, in1=vb[:, j, :],
                            op0=ALU.mult, op1=ALU.add), r=[ub.b, vecs.b], w=[vb.b])
            if dbg == 'v':
                dump(vb[:, :, :], vb.b)
            hs = hsave
            hsv = hs[:, :, :]
            T.op("act", lambda: nc.scalar.activation(out=hsv, in_=ub[:, :, TT:TT + HALO], func=AF.Copy),
                 r=[ub.b], w=[hs.b])
            zq = hT
            T.op("act", lambda: nc.scalar.activation(out=zq[:, :, :], in_=vb[:, :, :], func=AF.Copy),
                 r=[vb.b], w=[zq.b])
            T.op("act", lambda: nc.scalar.activation(out=sq[:, :, :], in_=vb[:, :, :], func=AF.Square),
                 r=[vb.b], w=[sq.b])
            p1 = ps()
            p2 = ps()
            for kc in range(KC):
                mm(p1[:, :], ones_bf[:, :], zq[:, kc, :], kc == 0, kc == KC - 1, r=[zq.b, ones_bf.b], w=[p1.b],
                   inc=(kc == KC - 1))
            for kc in range(KC):
                mm(p2[:, :], ones_bf[:, :], sq[:, kc, :], kc == 0, kc == KC - 1, r=[sq.b, ones_bf.b], w=[p2.b],
                   inc=(kc == KC - 1))
            mean = ltmp()
            rstd = ltmp()
            nmr = ltmp()
            T.op("dve", lambda: nc.vector.tensor_scalar(out=mean[:, :], in0=p1[:, :], scalar1=1.0 / D, scalar2=None,
                                                        op0=ALU.mult), r=[p1.b], w=[mean.b])
            T.op("dve", lambda: nc.vector.tensor_tensor(out=nmr[:, :], in0=mean[:, :], in1=mean[:, :], op=ALU.mult),
                 r=[mean.b], w=[nmr.b])
            T.op("dve", lambda: nc.vector.scalar_tensor_tensor(
                out=rstd[:, :], in0=p2[:, :], scalar=1.0 / D, in1=nmr[:, :], op0=ALU.mult, op1=ALU.subtract),
                r=[p2.b, nmr.b], w=[rstd.b])
            T.op("act", lambda: nc.scalar.activation(out=rstd[:, :], in_=rstd[:, :], func=AF.Sqrt, bias=LN_EPS,
                                                     scale=1.0), r=[rstd.b], w=[rstd.b])
            T.op("dve", lambda: nc.vector.reciprocal(out=rstd[:, :], in_=rstd[:, :]), r=[rstd.b], w=[rstd.b])
            T.op("dve", lambda: nc.vector.scalar_tensor_tensor(
                out=nmr[:, :], in0=mean[:, :], scalar=-1.0, in1=rstd[:, :], op0=ALU.mult, op1=ALU.mult),
                r=[mean.b, rstd.b], w=[nmr.b])
            for j in range(KC):
                t = tmp()
                T.op("dve", lambda: nc.vector.tensor_tensor(out=t[:, :], in0=vb[:, j, :], in1=rstd[:, :], op=ALU.mult),
                     r=[vb.b, rstd.b], w=[t.b])
                T.op("dve", lambda: nc.vector.tensor_tensor(out=t[:, :], in0=t[:, :], in1=nmr[:, :], op=ALU.add),
                     r=[t.b, nmr.b], w=[t.b])
                T.op("act", lambda: nc.scalar.activation(out=ub[:, j, HALO:HALO + TT], in_=t[:, :], func=AF.Silu,
                                                         bias=V(V_LNB + j), scale=V(V_LNG + j)),
                     r=[t.b, vecs.b], w=[ub.b])
            if dbg == 'z':
                dump(ub[:, :, HALO:HALO + TT], ub.b)
            for sgi in range(4):
                so = load_slab(wb["cwout"], 0, KC, sgi * 512)
                for j4 in range(4):
                    mc = sgi * 4 + j4
                    p = ps()
                    for kc in range(KC):
                        mm(p[:, :], so[:, kc, j4 * 128:(j4 + 1) * 128], ub[:, kc, HALO:HALO + TT], kc == 0,
                           kc == KC - 1, r=[so.b, ub.b], w=[p.b], inc=(kc == KC - 1))
                    t = tmp()
                    T.op("act", lambda: nc.scalar.activation(out=t[:, :], in_=p[:, :], func=AF.Identity,
                                                             bias=drv[:, 32 + mc:33 + mc],
                                                             scale=modT[:, 32 + mc:33 + mc]),
                         r=[p.b, drv.b, modT.b], w=[t.b])
                    T.op("dve", lambda: nc.vector.tensor_tensor(out=xs[:, mc, :], in0=xs[:, mc, :], in1=t[:, :],
                                                                op=ALU.add), r=[t.b, xs.b], w=[xs.b])
            if dbg == 'xa':
                dump(xs[:, :, :], xs.b)
            T.op("act", lambda: nc.scalar.activation(out=ub[:, :, 0:HALO], in_=hsv, func=AF.Copy),
                 r=[hs.b], w=[ub.b])
            rms_mod(DRV(16), MOD(0, 3))
            gate_up(wb["fwg"], wb["fwu"], FF // 512, actb)
            down_proj(wb["fwd"], FFC, actb, MOD(0, 5))
            T.dma("act", xview(x1T.t)[:, :, i * TT:(i + 1) * TT], xs[:, :, :], r=[xs.b], w=[x1T.b], accum=True)
        T.barrier()

    try:
        if front:
            phase_ab()
    except DbgStop:
        return nc
    if stage <= 1:
        for i in range(NT):
            T.dma("sp", xs[:, :, :], xview(x1T.t)[:, :, i * TT:(i + 1) * TT], r=[x1T.b], w=[xs.b])
            T.dma("sp", xview(outT.t)[:, :, i * TT:(i + 1) * TT], xs[:, :, :], r=[xs.b], w=[outT.b], accum=True)
        T.barrier()
        return nc

    def phase_c1():
        sq = sbr("sq1", [128, KC, TT], BF16, 0)
        cur["sq"] = sq
        qkb = [sbr(f"qkb{i}", [128, 4, TT], BF16, 16384 + i * 4096) for i in range(2)]
        cntq = 0
        for i in range(NT):
            T.dma("sp", xs[:, :, :], xview(x1T.t)[:, :, i * TT:(i + 1) * TT], r=[x1T.b], w=[xs.b])
            rms_mod(DRV(80), MOD(1, 0))
            for sgi in range(12):
                s = load_slab(wb["wqkv"], 0, KC, sgi * 512)
                ob = qkb[cntq % 2]
                cntq += 1
                if sgi < 8:
                    dst = qT_s if sgi < 4 else kT_s
                    scale = 0.125 if sgi < 4 else 1.0
                    for j4 in range(4):
                        p = ps()
                        for kc in range(KC):
                            mm(p[:, :], s[:, kc, j4 * 128:(j4 + 1) * 128], hT[:, kc, :], kc == 0, kc == KC - 1,
                               r=[s.b, hT.b], w=[p.b], inc=(kc == KC - 1))
                        if j4 % 2 == 0:
                            T.op("act", lambda: nc.scalar.activation(out=ob[:, j4, :], in_=p[:, :], func=AF.Copy,
                                                                     scale=scale), r=[p.b], w=[ob.b])
                        else:
                            T.op("dve", lambda: nc.vector.tensor_scalar(out=ob[:, j4, :], in0=p[:, :], scalar1=scale,
                                                                        scalar2=None, op0=ALU.mult),
                                 r=[p.b], w=[ob.b])
                    h0 = (sgi % 4) * 4
                    T.dma("act", dst.t[h0:h0 + 4, :, i * TT:(i + 1) * TT].rearrange("h p t -> p h t"), ob[:, :, :],
                          r=[ob.b], w=[dst.b], accum=True)
                else:
                    h0 = (sgi - 8) * 4
                    for tb in range(4):
                        p = ps()
                        for kc in range(KC):
                            mm(p[:, :], hT[:, kc, tb * 128:(tb + 1) * 128], s[:, kc, :], kc == 0, kc == KC - 1,
                               r=[s.b, hT.b], w=[p.b], inc=(kc == KC - 1))
                        if tb % 2 == 0:
                            T.op("act", lambda: nc.scalar.activation(out=ob[:, tb, :], in_=p[:, :], func=AF.Copy),
                                 r=[p.b], w=[ob.b])
                        else:
                            T.op("dve", lambda: nc.vector.tensor_copy(out=ob[:, tb, :], in_=p[:, :]),
                                 r=[p.b], w=[ob.b])
                    for hh in range(4):
                        T.dma("act", v_s.t[h0 + hh, :, i * 4:(i + 1) * 4, :], ob[:, :, hh * 128:(hh + 1) * 128],
                              r=[ob.b], w=[v_s.b], accum=True)
        T.barrier()

    def attention_tile(qi):
        q0 = qi * TT
        nkb = 4 * (qi + 1)
        klen = (qi + 1) * TT
        qb = [sbr(f"qb{i}", [128, TT], BF16, i * 1024) for i in range(2)]
        kbf = [sbr(f"kb{i}", [128, S], BF16, 2048 + i * 8192) for i in range(2)]
        vbf = [sbr(f"vb{i}", [128, S // 128, 128], BF16, 18432 + i * 8192) for i in range(2)]
        base = sbr("base", [128, S], F32, 34816)
        diag = sbr("diag", [128, H, 128], F32, 51200)
        pts = [sbr(f"pt{i}", [128, TT], BF16, 59392 + i * 1024) for i in range(4)]
        osq = sbr("osq", [128, TT], BF16, 63488)
        T.dma("sp", base[:, :], base_in[:, :], w=[base.b])
        T.dma("sp", xs[:, :, :], xview(x1T.t)[:, :, q0:q0 + TT], r=[x1T.b], w=[xs.b])
        for h in range(H):
            slope = 2.0 ** (-0.5 * (h + 1))
            T.op("dve", lambda: nc.vector.scalar_tensor_tensor(
                out=diag[:, h, :], in0=consts[:, C_ABSD:C_ABSD + 128], scalar=-slope,
                in1=consts[:, C_DMASK:C_DMASK + 128], op0=ALU.mult, op1=ALU.add), r=[consts.b], w=[diag.b])

        def load_head(h):
            T.dma("sp", qb[h % 2][:, :], qT_s.t[h, :, q0:q0 + TT], r=[qT_s.b], w=[qb[h % 2].b])
            T.dma("sp", kbf[h % 2][:, 0:klen], kT_s.t[h, :, 0:klen], r=[kT_s.b], w=[kbf[h % 2].b])
            T.dma("sp", vbf[h % 2][:, 0:nkb, :], v_s.t[h, :, 0:nkb, :], r=[v_s.b], w=[vbf[h % 2].b])

        load_head(0)
        ptc = 0
        U1, U2, Z1, Z2 = psb[0], psb[1], psb[2], psb[3]
        for h in range(H):
            if h + 1 < H:
                load_head(h + 1)
            slope = 2.0 ** (-0.5 * (h + 1))
            q_, k_, v_ = qb[h % 2], kbf[h % 2], vbf[h % 2]
            for kb in range(nkb):
                o = kb - 4 * qi
                c0 = max(0, o) * 128
                s1 = ps(4, 8)
                s2 = ps(4, 8)
                mm(s1[:, c0:TT], k_[0:64, kb * 128:(kb + 1) * 128], q_[0:64, c0:TT], True, True,
                   r=[k_.b, q_.b], w=[s1.b], inc=True)
                mm(s2[:, c0:TT], k_[64:128, kb * 128:(kb + 1) * 128], q_[64:128, c0:TT], True, True,
                   r=[k_.b, q_.b], w=[s2.b], inc=True)
                cur_pts = []
                for sx in (s1, s2):
                    t = tmp()
                    if o >= 0:
                        T.op("dve", lambda: nc.vector.tensor_tensor(out=t[:, c0:c0 + 128], in0=sx[:, c0:c0 + 128],
                                                                    in1=diag[:, h, :], op=ALU.add),
                             r=[sx.b, diag.b], w=[t.b])
                        if c0 + 128 < TT:
                            n = TT - c0 - 128
                            T.op("dve", lambda: nc.vector.scalar_tensor_tensor(
                                out=t[:, c0 + 128:TT], in0=base[:, 128:128 + n], scalar=-slope,
                                in1=sx[:, c0 + 128:TT], op0=ALU.mult, op1=ALU.add), r=[sx.b, base.b], w=[t.b])
                    else:
                        x0 = (4 * qi - kb) * 128
                        T.op("dve", lambda: nc.vector.scalar_tensor_tensor(
                            out=t[:, :], in0=base[:, x0:x0 + TT], scalar=-slope, in1=sx[:, :],
                            op0=ALU.mult, op1=ALU.add), r=[sx.b, base.b], w=[t.b])
                    pt = pts[ptc % 4]
                    ptc += 1
                    T.op("act", lambda: nc.scalar.activation(out=pt[:, c0:TT], in_=t[:, c0:TT], func=AF.Exp),
                         r=[t.b], w=[pt.b])
                    cur_pts.append(pt)
                first = kb == 0
                last = kb == nkb - 1
                mm(U1[:, c0:TT], v_[:, kb, :], cur_pts[0][:, c0:TT], first, last, r=[v_.b, cur_pts[0].b], w=[U1.b],
                   inc=last)
                mm(Z1[:, c0:TT], ones_bf[:, :], cur_pts[0][:, c0:TT], first, last, r=[ones_bf.b, cur_pts[0].b],
                   w=[Z1.b], inc=last)
                mm(U2[:, c0:TT], v_[:, kb, :], cur_pts[1][:, c0:TT], first, last, r=[v_.b, cur_pts[1].b], w=[U2.b],
                   inc=last)
                mm(Z2[:, c0:TT], ones_bf[:, :], cur_pts[1][:, c0:TT], first, last, r=[ones_bf.b, cur_pts[1].b],
                   w=[Z2.b], inc=True)
            r1 = tmp()
            T.op("dve", lambda: nc.vector.reciprocal(out=r1[:, :], in_=Z1[:, :]), r=[Z1.b], w=[r1.b])
            t1 = tmp()
            T.op("dve", lambda: nc.vector.tensor_tensor(out=t1[:, :], in0=U1[:, :], in1=r1[:, :], op=ALU.mult),
                 r=[U1.b, r1.b], w=[t1.b])
            r2 = tmp()
            T.op("dve", lambda: nc.vector.reciprocal(out=r2[:, :], in_=Z2[:, :]), r=[Z2.b], w=[r2.b])
            t2 = tmp()
            T.op("dve", lambda: nc.vector.tensor_tensor(out=t2[:, :], in0=U2[:, :], in1=r2[:, :], op=ALU.mult),
                 r=[U2.b, r2.b], w=[t2.b])
            ot = ltmp()
            T.op("dve", lambda: nc.vector.scalar_tensor_tensor(
                out=ot[:, :], in0=t2[:, :], scalar=drv[:, 161:162], in1=t1[:, :], op0=ALU.mult, op1=ALU.add),
                r=[t1.b, t2.b, drv.b], w=[ot.b])
            T.op("act", lambda: nc.scalar.activation(out=osq[:, :], in_=ot[:, :], func=AF.Square), r=[ot.b], w=[osq.b])
            pz = ps(4, 8)
            mm(pz[:, :], ones_bf[:, :], osq[:, :], True, True, r=[ones_bf.b, osq.b], w=[pz.b], inc=True)
            rs = ltmp()
            T.op("act", lambda: nc.scalar.activation(out=rs[:, :], in_=pz[:, :], func=AF.Sqrt, bias=RMS_EPS,
                                                     scale=1.0 / 128.0), r=[pz.b], w=[rs.b])
            T.op("dve", lambda: nc.vector.reciprocal(out=rs[:, :], in_=rs[:, :]), r=[rs.b], w=[rs.b])
            T.op("dve", lambda: nc.vector.tensor_tensor(out=ot[:, :], in0=ot[:, :], in1=rs[:, :], op=ALU.mult),
                 r=[ot.b, rs.b], w=[ot.b])
            T.op("act", lambda: nc.scalar.activation(out=hT[:, h, :], in_=ot[:, :], func=AF.Identity,
                                                     scale=drv[:, 166:167]), r=[ot.b, drv.b], w=[hT.b])
        if dbg == "attn_o" and qi == dbg_tile:
            dump(hT[:, :, :], hT.b, tile=qi)
        for sgi in range(4):
            s = load_slab(wb["wo"], 0, KC, sgi * 512)
            for j4 in range(4):
                mc = sgi * 4 + j4
                p = ps(4, 8)
                for kc in range(KC):
                    mm(p[:, :], s[:, kc, j4 * 128:(j4 + 1) * 128], hT[:, kc, :], kc == 0, kc == KC - 1,
                       r=[s.b, hT.b], w=[p.b], inc=(kc == KC - 1))
                G = MOD(1, 2)
                T.op("dve", lambda: nc.vector.scalar_tensor_tensor(
                    out=xs[:, mc, :], in0=p[:, :], scalar=G(mc), in1=xs[:, mc, :], op0=ALU.mult, op1=ALU.add),
                    r=[p.b, xs.b, modT.b], w=[xs.b])
        T.barrier()

    def moe_tile(qi):
        abuf = Buf("actD")
        actb = sbr("actD", [128, EFC, TT], BF16, 0, abuf)
        sq = sbr("sqD", [128, KC, TT], BF16, 0, abuf)
        cur["sq"] = sq
        combT = sbr("combT", [8, TT], F32, 57344)
        lgT = sbr("lgT", [8, TT], F32, 59392)
        sm = sbr("sm", [128, 256], F32, 61440)
        state = {}

        def post_chunk(c, t):
            if c == 0:
                state["pl"] = ps()
            pl = state["pl"]
            mm(pl[0:8, :], wr_sb[:, c, :], t[:, :], c == 0, c == KC - 1, r=[wr_sb.b, t.b], w=[pl.b], inc=True)

        if not front:
            T.dma("sp", xs[:, :, :], xview(x2T_in)[:, :, qi * TT:(qi + 1) * TT], w=[xs.b])
        rms_mod(DRV(96), MOD(1, 3), post_chunk)
        if (not front) and (not first):
            T.dma("sp", xs[:, :, :], xview(xacc_in)[:, :, qi * TT:(qi + 1) * TT], w=[xs.b])
        pl = state["pl"]
        T.op("act", lambda: nc.scalar.activation(out=lgT[0:8, :], in_=pl[0:8, :], func=AF.Copy), r=[pl.b], w=[lgT.b])
        for tb in range(4):
            o = tb * 64
            pt = ps()
            T.op("pe", lambda: nc.tensor.transpose(out=pt[:, 0:8], in_=lgT[0:8, tb * 128:(tb + 1) * 128],
                                                   identity=consts[0:8, C_ID:C_ID + 8]),
                 r=[lgT.b, consts.b], w=[pt.b])
            lg = sm[:, o:o + 8]
            eq1 = sm[:, o + 8:o + 16]
            l2 = sm[:, o + 16:o + 24]
            eq2 = sm[:, o + 24:o + 32]
            comb = sm[:, o + 32:o + 40]
            m1 = sm[:, o + 40:o + 41]
            m2 = sm[:, o + 41:o + 42]
            dlt = sm[:, o + 42:o + 43]
            w2 = sm[:, o + 43:o + 44]
            w1 = sm[:, o + 44:o + 45]
            sb_ = [sm.b]
            T.op("dve", lambda: nc.vector.tensor_copy(out=lg, in_=pt[:, 0:8]), r=[pt.b], w=sb_)
            T.op("dve", lambda: nc.vector.reduce_max(out=m1, in_=lg, axis=AX.X), r=sb_, w=sb_)
            T.op("dve", lambda: nc.vector.tensor_scalar(out=eq1, in0=lg, scalar1=m1, scalar2=None, op0=ALU.is_equal),
                 r=sb_, w=sb_)
            T.op("dve", lambda: nc.vector.scalar_tensor_tensor(out=l2, in0=eq1, scalar=-1e30, in1=lg, op0=ALU.mult,
                                                               op1=ALU.add), r=sb_, w=sb_)
            T.op("dve", lambda: nc.vector.reduce_max(out=m2, in_=l2, axis=AX.X), r=sb_, w=sb_)
            T.op("dve", lambda: nc.vector.tensor_scalar(out=eq2, in0=l2, scalar1=m2, scalar2=None, op0=ALU.is_equal),
                 r=sb_, w=sb_)
            T.op("dve", lambda: nc.vector.tensor_tensor(out=dlt, in0=m2, in1=m1, op=ALU.subtract), r=sb_, w=sb_)
            T.op("act", lambda: nc.scalar.activation(out=w2, in_=dlt, func=AF.Sigmoid), r=sb_, w=sb_)
            T.op("dve", lambda: nc.vector.tensor_scalar(out=w1, in0=w2, scalar1=-1.0, scalar2=1.0, op0=ALU.mult,
                                                        op1=ALU.add), r=sb_, w=sb_)
            T.op("dve", lambda: nc.vector.tensor_scalar(out=comb, in0=eq1, scalar1=w1, scalar2=None, op0=ALU.mult),
                 r=sb_, w=sb_)
            T.op("dve", lambda: nc.vector.scalar_tensor_tensor(out=comb, in0=eq2, scalar=w2, in1=comb, op0=ALU.mult,
                                                               op1=ALU.add), r=sb_, w=sb_)
            pt2 = ps()
            T.op("pe", lambda: nc.tensor.transpose(out=pt2[0:8, 0:128], in_=comb,
                                                   identity=consts[:, C_ID:C_ID + 128]),
                 r=[sm.b, consts.b], w=[pt2.b])
            T.op("act", lambda: nc.scalar.activation(out=combT[0:8, tb * 128:(tb + 1) * 128], in_=pt2[0:8, 0:128],
                                                     func=AF.Copy), r=[pt2.b], w=[combT.b])
        if dbg == "comb" and qi == dbg_tile:
            T.op("dve", lambda: nc.vector.memset(xs[:, 0:1, :], 0.0), w=[xs.b])
            T.op("act", lambda: nc.scalar.activation(out=xs[0:8, 0, :], in_=combT[0:8, :], func=AF.Copy),
                 r=[combT.b], w=[xs.b])
            dump(xs[:, :, :], xs.b, tile=qi)
        for e in range(ne_run):
            pcb = ps(4, 8)
            ge = e0 + e
            mm(pcb[:, :], consts[0:8, C_SEL + ge * 128:C_SEL + (ge + 1) * 128], combT[0:8, :], True, True,
               r=[consts.b, combT.b], w=[pcb.b], inc=True)
            cb = ltmp()
            T.op("act", lambda: nc.scalar.activation(out=cb[:, :], in_=pcb[:, :], func=AF.Copy), r=[pcb.b], w=[cb.b])
            gate_up(mwg_b[e], mwu_b[e], EF // 512, actb, cb=cb)
            down_proj(mwd_b[e], EFC, actb, MOD(1, 5), kgroup=14)
        if dbg == "moe1" and qi == dbg_tile:
            dump(xs[:, :, :], xs.b, tile=qi)
        if last:
            rstd = rms_stats(xs)
            for c in range(KC):
                T.op("dve", lambda: nc.vector.scalar_tensor_tensor(
                    out=xs[:, c, :], in0=xs[:, c, :], scalar=V(V_FING + c), in1=rstd[:, :], op0=ALU.mult,
                    op1=ALU.mult), r=[xs.b, rstd.b, vecs.b], w=[xs.b])
        T.dma("act", xview(outT.t)[:, :, qi * TT:(qi + 1) * TT], xs[:, :, :], r=[xs.b], w=[outT.b], accum=True)
        T.barrier()

    try:
        if front:
            phase_c1()
        for qi in range(NT):
            if dbg_tile is not None and qi != dbg_tile:
                continue
            if front:
                attention_tile(qi)
            if stage <= 2 or mode == "front":
                T.dma("act", xview(outT.t)[:, :, qi * TT:(qi + 1) * TT], xs[:, :, :], r=[xs.b], w=[outT.b],
                      accum=True)
                T.barrier()
            else:
                moe_tile(qi)
        if mode == "front":
            T.dma("sp", mvec_out.t[:, 0:192], modT[:, :], r=[modT.b], w=[mvec_out.b], accum=True)
            T.dma("sp", mvec_out.t[:, 192:368], drv[:, :], r=[drv.b], w=[mvec_out.b], accum=True)
            T.barrier()
    except DbgStop:
        return nc
    return nc


def _pm(v):
    v = np.asarray(v, np.float32).reshape(-1, 128)
    return np.ascontiguousarray(v.T)


def _host_consts():
    consts = np.zeros((128, C_TOT), np.float32)
    consts[:, C_ID:C_ID + 128] = np.eye(128, dtype=np.float32)
    kk = np.arange(128)[:, None]
    qq = np.arange(128)[None, :]
    consts[:, C_ABSD:C_ABSD + 128] = np.abs(qq - kk).astype(np.float32)
    allowed = (kk // 64) <= (qq // 64)
    consts[:, C_DMASK:C_DMASK + 128] = np.where(allowed, 0.0, NEG).astype(np.float32)
    for e in range(NE):
        consts[e, C_SEL + e * 128:C_SEL + (e + 1) * 128] = 1.0
    base = (np.arange(S)[None, :] - np.arange(128)[:, None]).astype(np.float32)
    return consts, np.ascontiguousarray(base)


def make_in_maps(inputs, cores, stage=99, ne_run=NE):
    f = lambda k: np.asarray(inputs[k], np.float32)
    x = f("x")
    c = f("c")
    consts, base = _host_consts()
    shared = {
        "consts": consts, "base": base,
        "mod_w": np.ascontiguousarray(f("mod_w")),
        "cwin": np.ascontiguousarray(f("conv_w_in")[0]),
        "cwout": np.ascontiguousarray(f("conv_w_out")[0]),
        "fwg": np.ascontiguousarray(f("ffn_w_gate")[0]),
        "fwu": np.ascontiguousarray(f("ffn_w_up")[0]),
        "fwd": np.ascontiguousarray(f("ffn_w_down")[0]),
        "wqkv": np.ascontiguousarray(f("attn_w_qkv")[0]),
        "wo": np.ascontiguousarray(f("attn_w_o")[0]),
        "wr": np.ascontiguousarray(f("moe_w_router")[0]),
    }
    if stage >= 3:
        shared["mwg"] = np.ascontiguousarray(f("moe_w_gate")[0][:ne_run])
        shared["mwu"] = np.ascontiguousarray(f("moe_w_up")[0][:ne_run])
        shared["mwd"] = np.ascontiguousarray(f("moe_w_down")[0][:ne_run])
    vecs = np.zeros((128, V_TOT), np.float32)
    for l in range(2):
        vecs[:, V_N1G + l * 16:V_N1G + (l + 1) * 16] = _pm(f("norm1_g")[l])
        vecs[:, V_N2G + l * 16:V_N2G + (l + 1) * 16] = _pm(f("norm2_g")[l])
        vecs[:, V_MODB + l * 96:V_MODB + (l + 1) * 96] = _pm(f("mod_b")[l])
    vecs[:, V_CBIN:V_CBIN + 32] = _pm(f("conv_b_in")[0])
    wdw = f("conv_w_dw")[0]
    vecs[:, V_CWDW:V_CWDW + 16 * CW] = np.ascontiguousarray(
        wdw.reshape(CW, 16, 128).transpose(2, 1, 0)).reshape(128, 16 * CW)
    vecs[:, V_CBDW:V_CBDW + 16] = _pm(f("conv_b_dw")[0])
    vecs[:, V_LNG:V_LNG + 16] = _pm(f("conv_ln_g")[0])
    vecs[:, V_LNB:V_LNB + 16] = _pm(f("conv_ln_b")[0])
    vecs[:, V_CBOUT:V_CBOUT + 16] = _pm(f("conv_b_out")[0])
    vecs[:, V_FING:V_FING + 16] = _pm(f("final_g"))
    vecs[:, V_SUBG] = f("attn_subln_g")[0]
    lam = np.concatenate([f("attn_lam_q1")[0], f("attn_lam_k1")[0], f("attn_lam_q2")[0], f("attn_lam_k2")[0]])
    vecs[:, V_LAM:V_LAM + 256] = lam[None, :]
    maps = []
    for b in cores:
        m = dict(shared)
        m["vecs"] = vecs
        m["xT"] = np.ascontiguousarray(x[b].T).reshape(KC, 128, S)
        m["cT"] = _pm(c[b])
        maps.append(m)
    return maps


def _launch(nc, maps, trace=False):
    names = set()
    for alloc in nc.allocations:
        try:
            if alloc.kind == "ExternalInput":
                names.add(alloc.memorylocations[0].name)
        except Exception:
            pass
    maps = [{k: v for k, v in m.items() if k in names} for m in maps]
    return run_bass_kernel_spmd(nc, maps, core_ids=list(range(len(maps))), trace=trace)


def run(inputs, stage=99, cores=None, trace=False, dbg=None, dbg_tile=None, ne_run=NE):
    cores = list(range(B)) if cores is None else cores
    nc = build(stage=stage, dbg=dbg, dbg_tile=dbg_tile, ne_run=ne_run)
    maps = make_in_maps(inputs, cores, stage, ne_run)
    res = _launch(nc, maps, trace)
    outs = [np.ascontiguousarray(r["outT"].reshape(D, S).T) for r in res.results]
    return np.stack(outs, axis=0), res


NE_PER_LAUNCH = 2


def run_multi(inputs, cores=None, trace=False):
    cores = list(range(B)) if cores is None else cores
    maps = make_in_maps(inputs, cores, stage=2)
    nc_f = build(stage=2, mode="front")
    res = _launch(nc_f, maps, trace)
    x2 = [r["outT"] for r in res.results]
    mvec = [r["mvec_out"] for r in res.results]
    acc = x2
    f = lambda k: np.asarray(inputs[k], np.float32)
    mwg, mwu, mwd = f("moe_w_gate")[0], f("moe_w_up")[0], f("moe_w_down")[0]
    nl = NE // NE_PER_LAUNCH
    progs = {}
    for k in range(nl):
        last = (k == nl - 1)
        if last not in progs:
            progs[last] = build(stage=3, mode="moe", e0=0, ne_run=NE_PER_LAUNCH, first=False, last=last)
        nc_m = progs[last]
        e0 = k * NE_PER_LAUNCH
        consts = maps[0]["consts"].copy()
        consts[:, C_SEL:] = 0.0
        for le in range(NE_PER_LAUNCH):
            consts[e0 + le, C_SEL + le * 128:C_SEL + (le + 1) * 128] = 1.0
        g = np.ascontiguousarray(mwg[e0:e0 + NE_PER_LAUNCH])
        u = np.ascontiguousarray(mwu[e0:e0 + NE_PER_LAUNCH])
        d = np.ascontiguousarray(mwd[e0:e0 + NE_PER_LAUNCH])
        mm_ = []
        for i in range(len(cores)):
            mm_.append({"vecs": maps[i]["vecs"], "consts": consts, "wr": maps[i]["wr"], "x2T": x2[i],
                        "xaccT": acc[i], "mvec": mvec[i], "mwg": g, "mwu": u, "mwd": d})
        res = _launch(nc_m, mm_, trace)
        acc = [r["outT"] for r in res.results]
    outs = [np.ascontiguousarray(a.reshape(D, S).T) for a in acc]
    return np.stack(outs, axis=0)


def kernel(**inputs):
    out = run_multi(inputs)
    return out.astype(np.float32)
```

```python
import math
import numpy as np
import concourse.bass as bass
import concourse.mybir as mybir
from concourse.bass_utils import run_bass_kernel_spmd

F32 = mybir.dt.float32
BF16 = mybir.dt.bfloat16
AF = mybir.ActivationFunctionType
ALU = mybir.AluOpType
AX = mybir.AxisListType

D = 2048
S = 4096
B = 8
KC = 16
TT = 512
NT = S // TT
FF = 5632
FFC = FF // 128
EF = 7168
EFC = EF // 128
NE = 8
H = 16
CW = 31
HALO = CW - 1
RMS_EPS = 1e-6
LN_EPS = 1e-5
NEG = -30000.0

V_N1G = 0
V_N2G = 32
V_MODB = 64
V_CBIN = 256
V_CWDW = 288
V_CBDW = 784
V_LNG = 800
V_LNB = 816
V_CBOUT = 832
V_FING = 848
V_SUBG = 864
V_LAM = 865
V_TOT = 1121
C_ID = 0
C_ABSD = 128
C_DMASK = 256
C_SEL = 384
C_TOT = 384 + 1024


def lam_init(layer_idx):
    return 0.8 - 0.6 * math.exp(-0.3 * layer_idx)


class Buf:
    __slots__ = ("name", "w", "r")

    def __init__(self, name):
        self.name = name
        self.w = {}
        self.r = {}


class Eng:
    def __init__(self, name, obj, sem, is_pe=False):
        self.name = name
        self.obj = obj
        self.sem = sem
        self.is_pe = is_pe
        self.count = 0
        self.pending = False
        self.waited = {}
        self.dma_sems = []
        self.dma_uses = []
        self.dma_next = 0


class Tracker:
    def __init__(self, nc, n_dma_sems=10):
        self.nc = nc
        self.sems = []

        def newsem(name):
            s = nc.alloc_semaphore(name)
            self.sems.append(s)
            return len(self.sems) - 1

        self.eng = {
            "pe": Eng("pe", nc.tensor, newsem("s_pe"), is_pe=True),
            "act": Eng("act", nc.scalar, newsem("s_act")),
            "dve": Eng("dve", nc.vector, newsem("s_dve")),
            "pool": Eng("pool", nc.gpsimd, newsem("s_pool")),
            "sp": Eng("sp", nc.sync, newsem("s_sp")),
        }
        for q in ("sp", "pool", "act"):
            e = self.eng[q]
            for i in range(n_dma_sems):
                e.dma_sems.append(newsem(f"d_{q}{i}"))
                e.dma_uses.append(0)

    def _wait(self, e, need):
        for s, v in need.items():
            if e.waited.get(s, 0) >= v:
                continue
            e.obj.wait_ge(self.sems[s], v)
            e.waited[s] = v

    def _need(self, e, r, w, nowaw=False):
        need = {}

        def add(d, allow_same):
            for s, v in d.items():
                if s == e.sem and not allow_same:
                    continue
                if need.get(s, 0) < v:
                    need[s] = v

        for b in r:
            add(b.w, not e.is_pe)
        for b in w:
            if not nowaw:
                add(b.w, not e.is_pe)
            add(b.r, False)
        return need

    def op(self, en, fn, r=(), w=(), inc=True):
        e = self.eng[en]
        need = self._need(e, r, w)
        if e.pending:
            assert need.get(e.sem, 0) <= e.count, "self-dep on pending token"
        self._wait(e, need)
        ins = fn()
        tok = e.count + 1
        if inc:
            ins.then_inc(self.sems[e.sem], 1)
            e.count = tok
            e.pending = False
        else:
            assert e.is_pe
            e.pending = True
        for b in r:
            if b.r.get(e.sem, 0) < tok:
                b.r[e.sem] = tok
        for b in w:
            b.w = {e.sem: tok}
            b.r = {}
        return ins

    def dma(self, q, out, in_, r=(), w=(), accum=False, **kw):
        e = self.eng[q]
        k = e.dma_next
        e.dma_next = (k + 1) % len(e.dma_sems)
        s = e.dma_sems[k]
        uses = e.dma_uses[k]
        need = self._need(e, r, w, nowaw=accum)
        if uses > 0 and need.get(s, 0) < 16 * uses:
            need[s] = 16 * uses
        self._wait(e, need)
        ins = e.obj.dma_start(out=out, in_=in_, **kw)
        ins.then_inc(self.sems[s], 16)
        e.dma_uses[k] = uses + 1
        tok = 16 * (uses + 1)
        for b in r:
            if b.r.get(s, 0) < tok:
                b.r[s] = tok
        for b in w:
            if accum:
                if b.w.get(s, 0) < tok:
                    b.w[s] = tok
            else:
                b.w = {s: tok}
                b.r = {}
        return ins

    def barrier(self, full=False):
        need = {}
        for e in self.eng.values():
            assert not e.pending
            if e.count > 0:
                need[e.sem] = e.count
            if e.name == "pool" and not full:
                continue
            for s, u in zip(e.dma_sems, e.dma_uses):
                if u > 0:
                    need[s] = 16 * u
        for e in self.eng.values():
            n2 = {s: v for s, v in need.items() if s != e.sem}
            self._wait(e, n2)


class Tile:
    def __init__(self, t, name):
        self.t = t
        self.b = Buf(name)

    def __getitem__(self, idx):
        return self.t[idx]


def build(stage=99, dbg=None, dbg_tile=None, ne_run=NE, mode="full", e0=0, first=True, last=True, x2out=False):
    nc = bass.Bass("TRN2", target_bir_lowering=False)
    T = Tracker(nc)

    def din(name, shape, dt=F32):
        return nc.dram_tensor(name, list(shape), dt, kind="ExternalInput").ap()

    def dscr(name, shape, dt):
        return Tile(nc.dram_tensor(name, list(shape), dt, kind="Internal").ap(), name)

    front = mode in ("full", "front")
    vecs_in = din("vecs", [128, V_TOT])
    consts_in = din("consts", [128, C_TOT])
    wr_in = din("wr", [D, NE])
    w_in = {}
    if front:
        xT_in = din("xT", [KC, 128, S])
        cT_in = din("cT", [128, KC])
        base_in = din("base", [128, S])
        mod_w = din("mod_w", [2, D, 6 * D])
        w_in = {
            "cwin": din("cwin", [D, 2 * D]),
            "cwout": din("cwout", [D, D]),
            "fwg": din("fwg", [D, FF]),
            "fwu": din("fwu", [D, FF]),
            "fwd": din("fwd", [FF, D]),
            "wqkv": din("wqkv", [D, 3 * D]),
            "wo": din("wo", [D, D]),
        }
    else:
        x2T_in = din("x2T", [KC, 128, S])
        mvec_in = din("mvec", [128, 368])
        if not first:
            xacc_in = din("xaccT", [KC, 128, S])
    if x2out:
        x2T_out = Tile(nc.dram_tensor("x2T_out", [KC, 128, S], F32, kind="ExternalOutput").ap(), "x2T_out")
    if mode == "front" or x2out:
        mvec_out = Tile(nc.dram_tensor("mvec_out", [128, 368], F32, kind="ExternalOutput").ap(), "mvec_out")
    if stage >= 3 and mode != "front":
        mwg_in = din("mwg", [ne_run, D, EF])
        mwu_in = din("mwu", [ne_run, D, EF])
        mwd_in = din("mwd", [ne_run, EF, D])
    outT = Tile(nc.dram_tensor("outT", [KC, 128, S], F32, kind="ExternalOutput").ap(), "outT")

    wb = {}
    for k, a in w_in.items():
        wb[k] = dscr("b_" + k, a.shape, BF16)
    mwg_b = [dscr(f"b_mwg{e}", [D, EF], BF16) for e in range(ne_run)]
    mwu_b = [dscr(f"b_mwu{e}", [D, EF], BF16) for e in range(ne_run)]
    mwd_b = [dscr(f"b_mwd{e}", [EF, D], BF16) for e in range(ne_run)]
    x1T = dscr("x1T", [KC, 128, S], F32)
    qT_s = dscr("qT_s", [H, 128, S], BF16)
    kT_s = dscr("kT_s", [H, 128, S], BF16)
    v_s = dscr("v_s", [H, 128, S // 128, 128], BF16)

    off = {"p": 16512}
    uid = {"n": 0}

    def _nbytes(shape, dt):
        n = 1
        for s_ in shape[1:]:
            n *= s_
        return n * (2 if dt == BF16 else 4)

    def sbp(name, shape, dt):
        t = nc.alloc_sbuf_tensor_at(name, list(shape), dt, offset=off["p"])
        off["p"] += (_nbytes(shape, dt) + 31) // 32 * 32
        return Tile(t, name)

    def ps_alloc(name):
        return Tile(nc.alloc_psum_tensor(name, [128, 512], F32), name)

    vecs = sbp("vecs", [128, V_TOT], F32)
    consts = sbp("consts", [128, C_TOT], F32)
    modT = sbp("modT", [128, 2 * 96], F32)
    drv = sbp("drv", [128, 176], F32)
    cvec = sbp("cvec", [128, 64], F32)
    ones_bf = sbp("ones_bf", [128, 128], BF16)
    wr_sb = sbp("wr_sb", [128, KC, NE], F32)
    xs = sbp("xs", [128, KC, TT], F32)
    hT = sbp("hT", [128, KC, TT], BF16)
    NSLAB = 4
    slabs = [sbp(f"slab{i}", [128, KC, 512], BF16) for i in range(NSLAB)]
    NTMP = 5
    tmps = [sbp(f"tmp{i}", [128, 512], F32) for i in range(NTMP)]
    NLT = 3
    ltmps = [sbp(f"ltmp{i}", [128, 512], F32) for i in range(NLT)]
    R0 = off["p"]
    RCAP = 229376 - R0
    assert RCAP >= 66496, RCAP

    def sbr(name, shape, dt, o, buf=None):
        assert o + _nbytes(shape, dt) <= RCAP, (name, o, _nbytes(shape, dt), RCAP)
        uid["n"] += 1
        t = nc.alloc_sbuf_tensor_at(f"{name}_{uid['n']}", list(shape), dt, offset=R0 + o)
        tl = Tile(t, name)
        if buf is not None:
            tl.b = buf
        return tl

    psb = [ps_alloc(f"ps{i}") for i in range(8)]
    st = {"slab": 0, "tmp": 0, "ps": 0, "lt": 0}

    def next_slab():
        s = slabs[st["slab"] % NSLAB]
        st["slab"] += 1
        return s

    def tmp():
        s = tmps[st["tmp"] % NTMP]
        st["tmp"] += 1
        return s

    def ltmp():
        s = ltmps[st["lt"] % NLT]
        st["lt"] += 1
        return s

    def ps(lo=0, hi=8):
        n = hi - lo
        s = psb[lo + st["ps"] % n]
        st["ps"] += 1
        return s

    def V(col, n=1):
        return vecs[:, col:col + n]

    def mm(out_ap, lhsT, rhs, start, stop, r, w, inc):
        return T.op("pe", lambda: nc.tensor.matmul(out_ap, lhsT=lhsT, rhs=rhs, start=start, stop=stop),
                    r=r, w=w, inc=inc)

    def load_slab(wt, kc0, nkc, c0, ncols=512):
        s = next_slab()
        src = wt.t.rearrange("(kc p) n -> p kc n", p=128)[:, kc0:kc0 + nkc, c0:c0 + ncols]
        T.dma("sp", s[:, 0:nkc, 0:ncols], src, r=[wt.b], w=[s.b])
        return s

    def xview(tl):
        return tl.rearrange("c p t -> p c t")

    def precast(src_ap, dst, K, N):
        RBK = 128
        for i in range(K // RBK):
            o = dst.t[i * RBK:(i + 1) * RBK, :].rearrange("k (a b) -> k a b", b=512)
            ii = src_ap[i * RBK:(i + 1) * RBK, :].rearrange("k (a b) -> k a b", b=512)
            T.dma("pool", o, ii, w=[dst.b], accum=True)

    T.dma("sp", vecs[:, :], vecs_in[:, :], w=[vecs.b])
    T.dma("sp", consts[:, :], consts_in[:, :], w=[consts.b])
    if front:
        T.dma("sp", cvec[:, 0:KC], cT_in[:, :], w=[cvec.b])
    else:
        T.dma("sp", modT[:, :], mvec_in[:, 0:192], w=[modT.b])
        T.dma("sp", drv[:, :], mvec_in[:, 192:368], w=[drv.b])
    T.dma("sp", wr_sb[:, :, :], wr_in.rearrange("(kc p) e -> p kc e", p=128), w=[wr_sb.b])
    T.op("dve", lambda: nc.vector.memset(ones_bf[:, :], 1.0), w=[ones_bf.b])

    for k in ("cwin", "cwout", "fwg", "fwu", "fwd", "wqkv", "wo"):
        if front and (stage >= 2 or k in ("cwin", "cwout", "fwg", "fwu", "fwd")):
            precast(w_in[k], wb[k], w_in[k].shape[0], w_in[k].shape[1])
    if stage >= 3 and mode != "front":
        for e in range(ne_run):
            precast(mwg_in[e], mwg_b[e], D, EF)
            precast(mwu_in[e], mwu_b[e], D, EF)
            precast(mwd_in[e], mwd_b[e], EF, D)

    def phase0():
        T.op("act", lambda: nc.scalar.activation(out=cvec[:, 16:32], in_=cvec[:, 0:16], func=AF.Silu),
             r=[cvec.b], w=[cvec.b])
        MSL = 512
        mws = [sbr("mw0", [128, KC, MSL], F32, 0), sbr("mw1", [128, KC, MSL], F32, 32768)]
        cnt = 0
        for l in range(2):
            pm = ps()
            for sgi in range(6 * D // MSL):
                mwt = mws[cnt % 2]
                cnt += 1
                src = mod_w[l].rearrange("(kc p) n -> p kc n", p=128)[:, :, sgi * MSL:(sgi + 1) * MSL]
                T.dma("sp", mwt[:, :, :], src, w=[mwt.b])
                for j4 in range(MSL // 128):
                    j = sgi * (MSL // 128) + j4
                    for kc in range(KC):
                        mm(pm[:, j:j + 1], mwt[:, kc, j4 * 128:(j4 + 1) * 128], cvec[:, 16 + kc:17 + kc],
                           kc == 0, kc == KC - 1, r=[mwt.b, cvec.b], w=[pm.b],
                           inc=(kc == KC - 1 and j4 == MSL // 128 - 1))
            T.op("dve", lambda: nc.vector.tensor_tensor(out=modT[:, l * 96:(l + 1) * 96], in0=pm[:, 0:96],
                                                        in1=V(V_MODB + l * 96, 96), op=ALU.add),
                 r=[pm.b, vecs.b], w=[modT.b])
        for l in range(2):
            o = l * 80
            m = l * 96
            T.op("dve", lambda: nc.vector.scalar_tensor_tensor(
                out=drv[:, o:o + 16], in0=modT[:, m + 16:m + 32], scalar=1.0, in1=V(V_N1G + l * 16, 16),
                op0=ALU.add, op1=ALU.mult), r=[modT.b, vecs.b], w=[drv.b])
            T.op("dve", lambda: nc.vector.scalar_tensor_tensor(
                out=drv[:, o + 16:o + 32], in0=modT[:, m + 64:m + 80], scalar=1.0, in1=V(V_N2G + l * 16, 16),
                op0=ALU.add, op1=ALU.mult), r=[modT.b, vecs.b], w=[drv.b])
        T.op("dve", lambda: nc.vector.tensor_tensor(out=drv[:, 32:48], in0=modT[:, 32:48], in1=V(V_CBOUT, 16),
                                                    op=ALU.mult), r=[modT.b, vecs.b], w=[drv.b])
        t0 = tmp()
        T.op("dve", lambda: nc.vector.tensor_tensor(out=t0[:, 0:64], in0=V(V_LAM, 64), in1=V(V_LAM + 64, 64),
                                                    op=ALU.mult), r=[vecs.b], w=[t0.b])
        T.op("dve", lambda: nc.vector.tensor_tensor(out=t0[:, 64:128], in0=V(V_LAM + 128, 64), in1=V(V_LAM + 192, 64),
                                                    op=ALU.mult), r=[vecs.b], w=[t0.b])
        T.op("dve", lambda: nc.vector.reduce_sum(out=drv[:, 162:163], in_=t0[:, 0:64], axis=AX.X), r=[t0.b], w=[drv.b])
        T.op("dve", lambda: nc.vector.reduce_sum(out=drv[:, 163:164], in_=t0[:, 64:128], axis=AX.X), r=[t0.b], w=[drv.b])
        T.op("act", lambda: nc.scalar.activation(out=drv[:, 164:166], in_=drv[:, 162:164], func=AF.Exp),
             r=[drv.b], w=[drv.b])
        T.op("dve", lambda: nc.vector.scalar_tensor_tensor(
            out=drv[:, 160:161], in0=drv[:, 164:165], scalar=lam_init(1), in1=drv[:, 165:166],
            op0=ALU.add, op1=ALU.subtract), r=[drv.b], w=[drv.b])
        T.op("dve", lambda: nc.vector.tensor_scalar(out=drv[:, 161:162], in0=drv[:, 160:161], scalar1=-1.0, scalar2=None,
                                                    op0=ALU.mult), r=[drv.b], w=[drv.b])
        T.op("dve", lambda: nc.vector.tensor_scalar(out=drv[:, 166:167], in0=V(V_SUBG, 1), scalar1=1.0 - lam_init(1),
                                                    scalar2=None, op0=ALU.mult), r=[vecs.b], w=[drv.b])
        T.barrier()


    if front:
        phase0()
    else:
        T.barrier()

    def MOD(l, which):
        m = l * 96 + which * 16
        return lambda c: modT[:, m + c:m + c + 1]

    def DRV(col):
        return lambda c: drv[:, col + c:col + c + 1]

    cur = {}

    def rms_stats(src3):
        sq = cur["sq"]
        T.op("act", lambda: nc.scalar.activation(out=sq[:, :, :], in_=src3[:, :, :], func=AF.Square),
             r=[src3.b], w=[sq.b])
        p = ps()
        for kc in range(KC):
            mm(p[:, :], ones_bf[:, :], sq[:, kc, :], kc == 0, kc == KC - 1, r=[sq.b, ones_bf.b], w=[p.b],
               inc=(kc == KC - 1))
        rstd = ltmp()
        T.op("act", lambda: nc.scalar.activation(out=rstd[:, :], in_=p[:, :], func=AF.Sqrt, bias=RMS_EPS,
                                                 scale=1.0 / D), r=[p.b], w=[rstd.b])
        T.op("dve", lambda: nc.vector.reciprocal(out=rstd[:, :], in_=rstd[:, :]), r=[rstd.b], w=[rstd.b])
        return rstd

    def rms_mod(A, Bsh, post_chunk=None):
        rstd = rms_stats(xs)
        for c in range(KC):
            t = tmp()
            T.op("dve", lambda: nc.vector.scalar_tensor_tensor(
                out=t[:, :], in0=xs[:, c, :], scalar=A(c), in1=rstd[:, :], op0=ALU.mult, op1=ALU.mult),
                r=[xs.b, rstd.b, drv.b, modT.b], w=[t.b])
            if post_chunk is None:
                T.op("act", lambda: nc.scalar.activation(out=hT[:, c, :], in_=t[:, :], func=AF.Identity,
                                                         bias=Bsh(c), scale=1.0),
                     r=[t.b, modT.b], w=[hT.b])
            else:
                T.op("dve", lambda: nc.vector.tensor_scalar(out=t[:, :], in0=t[:, :], scalar1=Bsh(c), scalar2=None,
                                                            op0=ALU.add), r=[t.b, modT.b], w=[t.b])
                T.op("act", lambda: nc.scalar.activation(out=hT[:, c, :], in_=t[:, :], func=AF.Copy),
                     r=[t.b], w=[hT.b])
                post_chunk(c, t)

    def down_proj(wt, nkc, actb, G, kgroup=KC):
        kgs = []
        k0 = 0
        while k0 < nkc:
            kgs.append((k0, min(kgroup, nkc - k0)))
            k0 += kgroup
        for cg in range(4):
            pbank = [ps(0, 4) for _ in range(4)]
            for gi, (k0, nk) in enumerate(kgs):
                s = load_slab(wt, k0, nk, cg * 512)
                for j4 in range(4):
                    p = pbank[j4]
                    for kk in range(nk):
                        first = (gi == 0 and kk == 0)
                        last = (gi == len(kgs) - 1 and kk == nk - 1)
                        mm(p[:, :], s[:, kk, j4 * 128:(j4 + 1) * 128], actb[:, k0 + kk, :], first, last,
                           r=[s.b, actb.b], w=[p.b], inc=(kk == nk - 1))
            for j4 in range(4):
                mc = cg * 4 + j4
                p = pbank[j4]
                T.op("dve", lambda: nc.vector.scalar_tensor_tensor(
                    out=xs[:, mc, :], in0=p[:, :], scalar=G(mc), in1=xs[:, mc, :], op0=ALU.mult, op1=ALU.add),
                    r=[p.b, xs.b, modT.b], w=[xs.b])

    def gate_up(wg, wu, nslab, actb, cb=None):
        for sgi in range(nslab):
            s1 = load_slab(wg, 0, KC, sgi * 512)
            s2 = load_slab(wu, 0, KC, sgi * 512)
            for j4 in range(4):
                j = sgi * 4 + j4
                pg = ps(4, 8)
                pu = ps(4, 8)
                for kc in range(KC):
                    mm(pg[:, :], s1[:, kc, j4 * 128:(j4 + 1) * 128], hT[:, kc, :], kc == 0, kc == KC - 1,
                       r=[s1.b, hT.b], w=[pg.b], inc=(kc == KC - 1))
                for kc in range(KC):
                    mm(pu[:, :], s2[:, kc, j4 * 128:(j4 + 1) * 128], hT[:, kc, :], kc == 0, kc == KC - 1,
                       r=[s2.b, hT.b], w=[pu.b], inc=(kc == KC - 1))
                sg_ = tmp()
                T.op("act", lambda: nc.scalar.activation(out=sg_[:, :], in_=pg[:, :], func=AF.Silu),
                     r=[pg.b], w=[sg_.b])
                if cb is not None:
                    T.op("dve", lambda: nc.vector.tensor_tensor(out=sg_[:, :], in0=sg_[:, :], in1=cb[:, :],
                                                                op=ALU.mult), r=[sg_.b, cb.b], w=[sg_.b])
                T.op("dve", lambda: nc.vector.tensor_tensor(out=actb[:, j, :], in0=pu[:, :], in1=sg_[:, :],
                                                            op=ALU.mult), r=[pu.b, sg_.b], w=[actb.b])


    class DbgStop(Exception):
        pass

    def dump(src3, buf, n=KC, tile=0):
        T.op("act", lambda: nc.scalar.activation(out=xs[:, 0:n, :], in_=src3, func=AF.Copy), r=[buf], w=[xs.b])
        T.dma("sp", xview(outT.t)[:, :, tile * TT:(tile + 1) * TT], xs[:, :, :], r=[xs.b], w=[outT.b], accum=True)
        T.barrier()
        raise DbgStop()

    def phase_ab():
        ub = sbr("ub", [128, KC, HALO + TT], BF16, 0)
        UO = 17344
        ubuf = Buf("U")
        vb = sbr("vb", [128, KC, TT], F32, UO, ubuf)
        sq = sbr("sq", [128, KC, TT], BF16, UO + 32768, ubuf)
        actb = sbr("actb", [128, FFC, TT], BF16, UO, ubuf)
        cur["sq"] = sq
        hsave = sbr("hsave", [128, KC, HALO], F32, UO + 49152)
        T.op("dve", lambda: nc.vector.memset(ub[:, :, 0:HALO], 0.0), w=[ub.b])
        for i in range(NT):
            T.dma("sp", xs[:, :, :], xview(xT_in)[:, :, i * TT:(i + 1) * TT], w=[xs.b])
            rms_mod(DRV(0), MOD(0, 0))
            if dbg == 'mod':
                T.op('act', lambda: nc.scalar.activation(out=xs[:, 0, 0:192], in_=modT[:, :], func=AF.Copy), r=[modT.b], w=[xs.b])
                T.op('act', lambda: nc.scalar.activation(out=xs[:, 1, 0:176], in_=drv[:, :], func=AF.Copy), r=[drv.b], w=[xs.b])
                dump(xs[:, :, :], xs.b)
            if dbg == 'h1':
                dump(hT[:, :, :], hT.b)
            for sgi in range(4):
                sv = load_slab(wb["cwin"], 0, KC, sgi * 512)
                sg = load_slab(wb["cwin"], 0, KC, D + sgi * 512)
                for j4 in range(4):
                    j = sgi * 4 + j4
                    pv = ps()
                    pg = ps()
                    for kc in range(KC):
                        mm(pv[:, :], sv[:, kc, j4 * 128:(j4 + 1) * 128], hT[:, kc, :], kc == 0, kc == KC - 1,
                           r=[sv.b, hT.b], w=[pv.b], inc=(kc == KC - 1))
                    for kc in range(KC):
                        mm(pg[:, :], sg[:, kc, j4 * 128:(j4 + 1) * 128], hT[:, kc, :], kc == 0, kc == KC - 1,
                           r=[sg.b, hT.b], w=[pg.b], inc=(kc == KC - 1))
                    sgm = tmp()
                    T.op("act", lambda: nc.scalar.activation(out=sgm[:, :], in_=pg[:, :], func=AF.Sigmoid,
                                                             bias=V(V_CBIN + 16 + j), scale=1.0),
                         r=[pg.b, vecs.b], w=[sgm.b])
                    T.op("dve", lambda: nc.vector.scalar_tensor_tensor(
                        out=ub[:, j, HALO:HALO + TT], in0=pv[:, :], scalar=V(V_CBIN + j), in1=sgm[:, :],
                        op0=ALU.add, op1=ALU.mult), r=[pv.b, sgm.b, vecs.b], w=[ub.b])
            if dbg == 'u':
                dump(ub[:, :, HALO:HALO + TT], ub.b)
            for j in range(KC):
                for wtap in range(CW):
                    wcol = V(V_CWDW + j * CW + wtap)
                    if wtap == 0:
                        T.op("dve", lambda: nc.vector.tensor_scalar(
                            out=vb[:, j, :], in0=ub[:, j, 0:TT], scalar1=wcol, scalar2=V(V_CBDW + j),
                            op0=ALU.mult, op1=ALU.add), r=[ub.b, vecs.b], w=[vb.b])
                    else:
                        T.op("dve", lambda: nc.vector.scalar_tensor_tensor(
                            out=vb[:, j, :], in0=ub[:, j, wtap:wtap + TT], scalar=wcol, in1=vb[:, j, :],
                            op0=ALU.mult, op1=ALU.add), r=[ub.b, vecs.b], w=[vb.b])
            if dbg == 'v':
                dump(vb[:, :, :], vb.b)
            hs = hsave
            hsv = hs[:, :, :]
            T.op("act", lambda: nc.scalar.activation(out=hsv, in_=ub[:, :, TT:TT + HALO], func=AF.Copy),
                 r=[ub.b], w=[hs.b])
            zq = hT
            T.op("act", lambda: nc.scalar.activation(out=zq[:, :, :], in_=vb[:, :, :], func=AF.Copy),
                 r=[vb.b], w=[zq.b])
            T.op("act", lambda: nc.scalar.activation(out=sq[:, :, :], in_=vb[:, :, :], func=AF.Square),
                 r=[vb.b], w=[sq.b])
            p1 = ps()
            p2 = ps()
            for kc in range(KC):
                mm(p1[:, :], ones_bf[:, :], zq[:, kc, :], kc == 0, kc == KC - 1, r=[zq.b, ones_bf.b], w=[p1.b],
                   inc=(kc == KC - 1))
            for kc in range(KC):
                mm(p2[:, :], ones_bf[:, :], sq[:, kc, :], kc == 0, kc == KC - 1, r=[sq.b, ones_bf.b], w=[p2.b],
                   inc=(kc == KC - 1))
            mean = ltmp()
            rstd = ltmp()
            nmr = ltmp()
            T.op("dve", lambda: nc.vector.tensor_scalar(out=mean[:, :], in0=p1[:, :], scalar1=1.0 / D, scalar2=None,
                                                        op0=ALU.mult), r=[p1.b], w=[mean.b])
            T.op("dve", lambda: nc.vector.tensor_tensor(out=nmr[:, :], in0=mean[:, :], in1=mean[:, :], op=ALU.mult),
                 r=[mean.b], w=[nmr.b])
            T.op("dve", lambda: nc.vector.scalar_tensor_tensor(
                out=rstd[:, :], in0=p2[:, :], scalar=1.0 / D, in1=nmr[:, :], op0=ALU.mult, op1=ALU.subtract),
                r=[p2.b, nmr.b], w=[rstd.b])
            T.op("act", lambda: nc.scalar.activation(out=rstd[:, :], in_=rstd[:, :], func=AF.Sqrt, bias=LN_EPS,
                                                     scale=1.0), r=[rstd.b], w=[rstd.b])
            T.op("dve", lambda: nc.vector.reciprocal(out=rstd[:, :], in_=rstd[:, :]), r=[rstd.b], w=[rstd.b])
            T.op("dve", lambda: nc.vector.scalar_tensor_tensor(
                out=nmr[:, :], in0=mean[:, :], scalar=-1.0, in1=rstd[:, :], op0=ALU.mult, op1=ALU.mult),
                r=[mean.b, rstd.b], w=[nmr.b])
            for j in range(KC):
                t = tmp()
                T.op("dve", lambda: nc.vector.tensor_tensor(out=t[:, :], in0=vb[:, j, :], in1=rstd[:, :], op=ALU.mult),
                     r=[vb.b, rstd.b], w=[t.b])
                T.op("dve", lambda: nc.vector.tensor_tensor(out=t[:, :], in0=t[:, :], in1=nmr[:, :], op=ALU.add),
                     r=[t.b, nmr.b], w=[t.b])
                T.op("act", lambda: nc.scalar.activation(out=ub[:, j, HALO:HALO + TT], in_=t[:, :], func=AF.Silu,
                                                         bias=V(V_LNB + j), scale=V(V_LNG + j)),
                     r=[t.b, vecs.b], w=[ub.b])
            if dbg == 'z':
                dump(ub[:, :, HALO:HALO + TT], ub.b)
            for sgi in range(4):
                so = load_slab(wb["cwout"], 0, KC, sgi * 512)
                for j4 in range(4):
                    mc = sgi * 4 + j4
                    p = ps()
                    for kc in range(KC):
                        mm(p[:, :], so[:, kc, j4 * 128:(j4 + 1) * 128], ub[:, kc, HALO:HALO + TT], kc == 0,
                           kc == KC - 1, r=[so.b, ub.b], w=[p.b], inc=(kc == KC - 1))
                    t = tmp()
                    T.op("act", lambda: nc.scalar.activation(out=t[:, :], in_=p[:, :], func=AF.Identity,
                                                             bias=drv[:, 32 + mc:33 + mc],
                                                             scale=modT[:, 32 + mc:33 + mc]),
                         r=[p.b, drv.b, modT.b], w=[t.b])
                    T.op("dve", lambda: nc.vector.tensor_tensor(out=xs[:, mc, :], in0=xs[:, mc, :], in1=t[:, :],
                                                                op=ALU.add), r=[t.b, xs.b], w=[xs.b])
            if dbg == 'xa':
                dump(xs[:, :, :], xs.b)
            T.op("act", lambda: nc.scalar.activation(out=ub[:, :, 0:HALO], in_=hsv, func=AF.Copy),
                 r=[hs.b], w=[ub.b])
            rms_mod(DRV(16), MOD(0, 3))
            gate_up(wb["fwg"], wb["fwu"], FF // 512, actb)
            down_proj(wb["fwd"], FFC, actb, MOD(0, 5))
            T.dma("act", xview(x1T.t)[:, :, i * TT:(i + 1) * TT], xs[:, :, :], r=[xs.b], w=[x1T.b], accum=True)
        T.barrier()

    try:
        if front:
            phase_ab()
    except DbgStop:
        return nc
    if stage <= 1:
        for i in range(NT):
            T.dma("sp", xs[:, :, :], xview(x1T.t)[:, :, i * TT:(i + 1) * TT], r=[x1T.b], w=[xs.b])
            T.dma("sp", xview(outT.t)[:, :, i * TT:(i + 1) * TT], xs[:, :, :], r=[xs.b], w=[outT.b], accum=True)
        T.barrier()
        return nc

    def phase_c1():
        sq = sbr("sq1", [128, KC, TT], BF16, 0)
        cur["sq"] = sq
        qkb = [sbr(f"qkb{i}", [128, 4, TT], BF16, 16384 + i * 4096) for i in range(2)]
        cntq = 0
        for i in range(NT):
            T.dma("sp", xs[:, :, :], xview(x1T.t)[:, :, i * TT:(i + 1) * TT], r=[x1T.b], w=[xs.b])
            rms_mod(DRV(80), MOD(1, 0))
            for sgi in range(12):
                s = load_slab(wb["wqkv"], 0, KC, sgi * 512)
                ob = qkb[cntq % 2]
                cntq += 1
                if sgi < 8:
                    dst = qT_s if sgi < 4 else kT_s
                    scale = 0.125 if sgi < 4 else 1.0
                    for j4 in range(4):
                        p = ps()
                        for kc in range(KC):
                            mm(p[:, :], s[:, kc, j4 * 128:(j4 + 1) * 128], hT[:, kc, :], kc == 0, kc == KC - 1,
                               r=[s.b, hT.b], w=[p.b], inc=(kc == KC - 1))
                        if j4 % 2 == 0:
                            T.op("act", lambda: nc.scalar.activation(out=ob[:, j4, :], in_=p[:, :], func=AF.Copy,
                                                                     scale=scale), r=[p.b], w=[ob.b])
                        else:
                            T.op("dve", lambda: nc.vector.tensor_scalar(out=ob[:, j4, :], in0=p[:, :], scalar1=scale,
                                                                        scalar2=None, op0=ALU.mult),
                                 r=[p.b], w=[ob.b])
                    h0 = (sgi % 4) * 4
                    T.dma("act", dst.t[h0:h0 + 4, :, i * TT:(i + 1) * TT].rearrange("h p t -> p h t"), ob[:, :, :],
                          r=[ob.b], w=[dst.b], accum=True)
                else:
                    h0 = (sgi - 8) * 4
                    for tb in range(4):
                        p = ps()
                        for kc in range(KC):
                            mm(p[:, :], hT[:, kc, tb * 128:(tb + 1) * 128], s[:, kc, :], kc == 0, kc == KC - 1,
                               r=[s.b, hT.b], w=[p.b], inc=(kc == KC - 1))
                        if tb % 2 == 0:
                            T.op("act", lambda: nc.scalar.activation(out=ob[:, tb, :], in_=p[:, :], func=AF.Copy),
                                 r=[p.b], w=[ob.b])
                        else:
                            T.op("dve", lambda: nc.vector.tensor_copy(out=ob[:, tb, :], in_=p[:, :]),
                                 r=[p.b], w=[ob.b])
                    for hh in range(4):
                        T.dma("act", v_s.t[h0 + hh, :, i * 4:(i + 1) * 4, :], ob[:, :, hh * 128:(hh + 1) * 128],
                              r=[ob.b], w=[v_s.b], accum=True)
        T.barrier()

    def attention_tile(qi):
        q0 = qi * TT
        nkb = 4 * (qi + 1)
        klen = (qi + 1) * TT
        qb = [sbr(f"qb{i}", [128, TT], BF16, i * 1024) for i in range(2)]
        kbf = [sbr(f"kb{i}", [128, S], BF16, 2048 + i * 8192) for i in range(2)]
        vbf = [sbr(f"vb{i}", [128, S // 128, 128], BF16, 18432 + i * 8192) for i in range(2)]
        base = sbr("base", [128, S], F32, 34816)
        diag = sbr("diag", [128, H, 128], F32, 51200)
        pts = [sbr(f"pt{i}", [128, TT], BF16, 59392 + i * 1024) for i in range(4)]
        osq = sbr("osq", [128, TT], BF16, 63488)
        T.dma("sp", base[:, :], base_in[:, :], w=[base.b])
        T.dma("sp", xs[:, :, :], xview(x1T.t)[:, :, q0:q0 + TT], r=[x1T.b], w=[xs.b])
        for h in range(H):
            slope = 2.0 ** (-0.5 * (h + 1))
            T.op("dve", lambda: nc.vector.scalar_tensor_tensor(
                out=diag[:, h, :], in0=consts[:, C_ABSD:C_ABSD + 128], scalar=-slope,
                in1=consts[:, C_DMASK:C_DMASK + 128], op0=ALU.mult, op1=ALU.add), r=[consts.b], w=[diag.b])

        def load_head(h):
            T.dma("sp", qb[h % 2][:, :], qT_s.t[h, :, q0:q0 + TT], r=[qT_s.b], w=[qb[h % 2].b])
            T.dma("sp", kbf[h % 2][:, 0:klen], kT_s.t[h, :, 0:klen], r=[kT_s.b], w=[kbf[h % 2].b])
            T.dma("sp", vbf[h % 2][:, 0:nkb, :], v_s.t[h, :, 0:nkb, :], r=[v_s.b], w=[vbf[h % 2].b])

        load_head(0)
        ptc = 0
        U1, U2, Z1, Z2 = psb[0], psb[1], psb[2], psb[3]
        for h in range(H):
            if h + 1 < H:
                load_head(h + 1)
            slope = 2.0 ** (-0.5 * (h + 1))
            q_, k_, v_ = qb[h % 2], kbf[h % 2], vbf[h % 2]
            def scores(kb):
                o = kb - 4 * qi
                c0 = max(0, o) * 128
                s1 = ps(4, 8)
                s2 = ps(4, 8)
                mm(s1[:, c0:TT], k_[0:64, kb * 128:(kb + 1) * 128], q_[0:64, c0:TT], True, True,
                   r=[k_.b, q_.b], w=[s1.b], inc=True)
                mm(s2[:, c0:TT], k_[64:128, kb * 128:(kb + 1) * 128], q_[64:128, c0:TT], True, True,
                   r=[k_.b, q_.b], w=[s2.b], inc=True)
                return (s1, s2, c0, o)

            sc = scores(0)
            for kb in range(nkb):
                s1, s2, c0, o = sc
                sc = scores(kb + 1) if kb + 1 < nkb else None
                cur_pts = []
                for sx in (s1, s2):
                    t = tmp()
                    if o >= 0:
                        T.op("dve", lambda: nc.vector.tensor_tensor(out=t[:, c0:c0 + 128], in0=sx[:, c0:c0 + 128],
                                                                    in1=diag[:, h, :], op=ALU.add),
                             r=[sx.b, diag.b], w=[t.b])
                        if c0 + 128 < TT:
                            n = TT - c0 - 128
                            T.op("dve", lambda: nc.vector.scalar_tensor_tensor(
                                out=t[:, c0 + 128:TT], in0=base[:, 128:128 + n], scalar=-slope,
                                in1=sx[:, c0 + 128:TT], op0=ALU.mult, op1=ALU.add), r=[sx.b, base.b], w=[t.b])
                    else:
                        x0 = (4 * qi - kb) * 128
                        T.op("dve", lambda: nc.vector.scalar_tensor_tensor(
                            out=t[:, :], in0=base[:, x0:x0 + TT], scalar=-slope, in1=sx[:, :],
                            op0=ALU.mult, op1=ALU.add), r=[sx.b, base.b], w=[t.b])
                    pt = pts[ptc % 4]
                    ptc += 1
                    T.op("act", lambda: nc.scalar.activation(out=pt[:, c0:TT], in_=t[:, c0:TT], func=AF.Exp),
                         r=[t.b], w=[pt.b])
                    cur_pts.append(pt)
                first_ = kb == 0
                last_ = kb == nkb - 1
                mm(U1[:, c0:TT], v_[:, kb, :], cur_pts[0][:, c0:TT], first_, last_, r=[v_.b, cur_pts[0].b],
                   w=[U1.b], inc=last_)
                mm(Z1[:, c0:TT], ones_bf[:, :], cur_pts[0][:, c0:TT], first_, last_, r=[ones_bf.b, cur_pts[0].b],
                   w=[Z1.b], inc=last_)
                mm(U2[:, c0:TT], v_[:, kb, :], cur_pts[1][:, c0:TT], first_, last_, r=[v_.b, cur_pts[1].b],
                   w=[U2.b], inc=last_)
                mm(Z2[:, c0:TT], ones_bf[:, :], cur_pts[1][:, c0:TT], first_, last_, r=[ones_bf.b, cur_pts[1].b],
                   w=[Z2.b], inc=True)
            r1 = tmp()
            T.op("dve", lambda: nc.vector.reciprocal(out=r1[:, :], in_=Z1[:, :]), r=[Z1.b], w=[r1.b])
            t1 = tmp()
            T.op("dve", lambda: nc.vector.tensor_tensor(out=t1[:, :], in0=U1[:, :], in1=r1[:, :], op=ALU.mult),
                 r=[U1.b, r1.b], w=[t1.b])
            r2 = tmp()
            T.op("dve", lambda: nc.vector.reciprocal(out=r2[:, :], in_=Z2[:, :]), r=[Z2.b], w=[r2.b])
            t2 = tmp()
            T.op("dve", lambda: nc.vector.tensor_tensor(out=t2[:, :], in0=U2[:, :], in1=r2[:, :], op=ALU.mult),
                 r=[U2.b, r2.b], w=[t2.b])
            ot = ltmp()
            T.op("dve", lambda: nc.vector.scalar_tensor_tensor(
                out=ot[:, :], in0=t2[:, :], scalar=drv[:, 161:162], in1=t1[:, :], op0=ALU.mult, op1=ALU.add),
                r=[t1.b, t2.b, drv.b], w=[ot.b])
            T.op("act", lambda: nc.scalar.activation(out=osq[:, :], in_=ot[:, :], func=AF.Square), r=[ot.b], w=[osq.b])
            pz = ps(4, 8)
            st["ps"] += 1
            mm(pz[:, :], ones_bf[:, :], osq[:, :], True, True, r=[ones_bf.b, osq.b], w=[pz.b], inc=True)
            rs = ltmp()
            T.op("act", lambda: nc.scalar.activation(out=rs[:, :], in_=pz[:, :], func=AF.Sqrt, bias=RMS_EPS,
                                                     scale=1.0 / 128.0), r=[pz.b], w=[rs.b])
            T.op("dve", lambda: nc.vector.reciprocal(out=rs[:, :], in_=rs[:, :]), r=[rs.b], w=[rs.b])
            T.op("dve", lambda: nc.vector.tensor_tensor(out=ot[:, :], in0=ot[:, :], in1=rs[:, :], op=ALU.mult),
                 r=[ot.b, rs.b], w=[ot.b])
            T.op("act", lambda: nc.scalar.activation(out=hT[:, h, :], in_=ot[:, :], func=AF.Identity,
                                                     scale=drv[:, 166:167]), r=[ot.b, drv.b], w=[hT.b])
        if dbg == "attn_o" and qi == dbg_tile:
            dump(hT[:, :, :], hT.b, tile=qi)
        for sgi in range(4):
            s = load_slab(wb["wo"], 0, KC, sgi * 512)
            for j4 in range(4):
                mc = sgi * 4 + j4
                p = ps(4, 8)
                for kc in range(KC):
                    mm(p[:, :], s[:, kc, j4 * 128:(j4 + 1) * 128], hT[:, kc, :], kc == 0, kc == KC - 1,
                       r=[s.b, hT.b], w=[p.b], inc=(kc == KC - 1))
                G = MOD(1, 2)
                T.op("dve", lambda: nc.vector.scalar_tensor_tensor(
                    out=xs[:, mc, :], in0=p[:, :], scalar=G(mc), in1=xs[:, mc, :], op0=ALU.mult, op1=ALU.add),
                    r=[p.b, xs.b, modT.b], w=[xs.b])
        T.barrier()

    def moe_tile(qi):
        abuf = Buf("actD")
        actb = sbr("actD", [128, EFC, TT], BF16, 0, abuf)
        sq = sbr("sqD", [128, KC, TT], BF16, 0, abuf)
        cur["sq"] = sq
        combT = sbr("combT", [8, TT], F32, 57344)
        lgT = sbr("lgT", [8, TT], F32, 59392)
        sm = sbr("sm", [128, 256], F32, 61440)
        state = {}

        def post_chunk(c, t):
            if c == 0:
                state["pl"] = ps()
            pl = state["pl"]
            mm(pl[0:8, :], wr_sb[:, c, :], t[:, :], c == 0, c == KC - 1, r=[wr_sb.b, t.b], w=[pl.b], inc=True)

        if not front:
            T.dma("sp", xs[:, :, :], xview(x2T_in)[:, :, qi * TT:(qi + 1) * TT], w=[xs.b])
        rms_mod(DRV(96), MOD(1, 3), post_chunk)
        if (not front) and (not first):
            T.dma("sp", xs[:, :, :], xview(xacc_in)[:, :, qi * TT:(qi + 1) * TT], w=[xs.b])
        pl = state["pl"]
        T.op("act", lambda: nc.scalar.activation(out=lgT[0:8, :], in_=pl[0:8, :], func=AF.Copy), r=[pl.b], w=[lgT.b])
        for tb in range(4):
            o = tb * 64
            pt = ps()
            T.op("pe", lambda: nc.tensor.transpose(out=pt[:, 0:8], in_=lgT[0:8, tb * 128:(tb + 1) * 128],
                                                   identity=consts[0:8, C_ID:C_ID + 8]),
                 r=[lgT.b, consts.b], w=[pt.b])
            lg = sm[:, o:o + 8]
            eq1 = sm[:, o + 8:o + 16]
            l2 = sm[:, o + 16:o + 24]
            eq2 = sm[:, o + 24:o + 32]
            comb = sm[:, o + 32:o + 40]
            m1 = sm[:, o + 40:o + 41]
            m2 = sm[:, o + 41:o + 42]
            dlt = sm[:, o + 42:o + 43]
            w2 = sm[:, o + 43:o + 44]
            w1 = sm[:, o + 44:o + 45]
            sb_ = [sm.b]
            T.op("dve", lambda: nc.vector.tensor_copy(out=lg, in_=pt[:, 0:8]), r=[pt.b], w=sb_)
            T.op("dve", lambda: nc.vector.reduce_max(out=m1, in_=lg, axis=AX.X), r=sb_, w=sb_)
            T.op("dve", lambda: nc.vector.tensor_scalar(out=eq1, in0=lg, scalar1=m1, scalar2=None, op0=ALU.is_equal),
                 r=sb_, w=sb_)
            T.op("dve", lambda: nc.vector.scalar_tensor_tensor(out=l2, in0=eq1, scalar=-1e30, in1=lg, op0=ALU.mult,
                                                               op1=ALU.add), r=sb_, w=sb_)
            T.op("dve", lambda: nc.vector.reduce_max(out=m2, in_=l2, axis=AX.X), r=sb_, w=sb_)
            T.op("dve", lambda: nc.vector.tensor_scalar(out=eq2, in0=l2, scalar1=m2, scalar2=None, op0=ALU.is_equal),
                 r=sb_, w=sb_)
            T.op("dve", lambda: nc.vector.tensor_tensor(out=dlt, in0=m2, in1=m1, op=ALU.subtract), r=sb_, w=sb_)
            T.op("act", lambda: nc.scalar.activation(out=w2, in_=dlt, func=AF.Sigmoid), r=sb_, w=sb_)
            T.op("dve", lambda: nc.vector.tensor_scalar(out=w1, in0=w2, scalar1=-1.0, scalar2=1.0, op0=ALU.mult,
                                                        op1=ALU.add), r=sb_, w=sb_)
            T.op("dve", lambda: nc.vector.tensor_scalar(out=comb, in0=eq1, scalar1=w1, scalar2=None, op0=ALU.mult),
                 r=sb_, w=sb_)
            T.op("dve", lambda: nc.vector.scalar_tensor_tensor(out=comb, in0=eq2, scalar=w2, in1=comb, op0=ALU.mult,
                                                               op1=ALU.add), r=sb_, w=sb_)
            pt2 = ps()
            T.op("pe", lambda: nc.tensor.transpose(out=pt2[0:8, 0:128], in_=comb,
                                                   identity=consts[:, C_ID:C_ID + 128]),
                 r=[sm.b, consts.b], w=[pt2.b])
            T.op("act", lambda: nc.scalar.activation(out=combT[0:8, tb * 128:(tb + 1) * 128], in_=pt2[0:8, 0:128],
                                                     func=AF.Copy), r=[pt2.b], w=[combT.b])
        if dbg == "comb" and qi == dbg_tile:
            T.op("dve", lambda: nc.vector.memset(xs[:, 0:1, :], 0.0), w=[xs.b])
            T.op("act", lambda: nc.scalar.activation(out=xs[0:8, 0, :], in_=combT[0:8, :], func=AF.Copy),
                 r=[combT.b], w=[xs.b])
            dump(xs[:, :, :], xs.b, tile=qi)
        for e in range(ne_run):
            pcb = ps(4, 8)
            ge = e0 + e
            mm(pcb[:, :], consts[0:8, C_SEL + ge * 128:C_SEL + (ge + 1) * 128], combT[0:8, :], True, True,
               r=[consts.b, combT.b], w=[pcb.b], inc=True)
            cb = ltmp()
            T.op("act", lambda: nc.scalar.activation(out=cb[:, :], in_=pcb[:, :], func=AF.Copy), r=[pcb.b], w=[cb.b])
            gate_up(mwg_b[e], mwu_b[e], EF // 512, actb, cb=cb)
            down_proj(mwd_b[e], EFC, actb, MOD(1, 5), kgroup=14)
        if dbg == "moe1" and qi == dbg_tile:
            dump(xs[:, :, :], xs.b, tile=qi)
        if last:
            rstd = rms_stats(xs)
            for c in range(KC):
                T.op("dve", lambda: nc.vector.scalar_tensor_tensor(
                    out=xs[:, c, :], in0=xs[:, c, :], scalar=V(V_FING + c), in1=rstd[:, :], op0=ALU.mult,
                    op1=ALU.mult), r=[xs.b, rstd.b, vecs.b], w=[xs.b])
        T.dma("act", xview(outT.t)[:, :, qi * TT:(qi + 1) * TT], xs[:, :, :], r=[xs.b], w=[outT.b], accum=True)
        T.barrier()

    try:
        if front:
            phase_c1()
        for qi in range(NT):
            if dbg_tile is not None and qi != dbg_tile:
                continue
            if front:
                attention_tile(qi)
                if x2out:
                    T.dma("act", xview(x2T_out.t)[:, :, qi * TT:(qi + 1) * TT], xs[:, :, :], r=[xs.b],
                          w=[x2T_out.b], accum=True)
            if stage <= 2 or mode == "front":
                T.dma("act", xview(outT.t)[:, :, qi * TT:(qi + 1) * TT], xs[:, :, :], r=[xs.b], w=[outT.b],
                      accum=True)
                T.barrier()
            else:
                moe_tile(qi)
        if mode == "front" or x2out:
            T.dma("sp", mvec_out.t[:, 0:192], modT[:, :], r=[modT.b], w=[mvec_out.b], accum=True)
            T.dma("sp", mvec_out.t[:, 192:368], drv[:, :], r=[drv.b], w=[mvec_out.b], accum=True)
        T.barrier(full=True)
    except DbgStop:
        return nc
    return nc


def _pm(v):
    v = np.asarray(v, np.float32).reshape(-1, 128)
    return np.ascontiguousarray(v.T)


def _host_consts():
    consts = np.zeros((128, C_TOT), np.float32)
    consts[:, C_ID:C_ID + 128] = np.eye(128, dtype=np.float32)
    kk = np.arange(128)[:, None]
    qq = np.arange(128)[None, :]
    consts[:, C_ABSD:C_ABSD + 128] = np.abs(qq - kk).astype(np.float32)
    allowed = (kk // 64) <= (qq // 64)
    consts[:, C_DMASK:C_DMASK + 128] = np.where(allowed, 0.0, NEG).astype(np.float32)
    for e in range(NE):
        consts[e, C_SEL + e * 128:C_SEL + (e + 1) * 128] = 1.0
    base = (np.arange(S)[None, :] - np.arange(128)[:, None]).astype(np.float32)
    return consts, np.ascontiguousarray(base)


def make_in_maps(inputs, cores, stage=99, ne_run=NE):
    f = lambda k: np.asarray(inputs[k], np.float32)
    x = f("x")
    c = f("c")
    consts, base = _host_consts()
    shared = {
        "consts": consts, "base": base,
        "mod_w": np.ascontiguousarray(f("mod_w")),
        "cwin": np.ascontiguousarray(f("conv_w_in")[0]),
        "cwout": np.ascontiguousarray(f("conv_w_out")[0]),
        "fwg": np.ascontiguousarray(f("ffn_w_gate")[0]),
        "fwu": np.ascontiguousarray(f("ffn_w_up")[0]),
        "fwd": np.ascontiguousarray(f("ffn_w_down")[0]),
        "wqkv": np.ascontiguousarray(f("attn_w_qkv")[0]),
        "wo": np.ascontiguousarray(f("attn_w_o")[0]),
        "wr": np.ascontiguousarray(f("moe_w_router")[0]),
    }
    if stage >= 3:
        shared["mwg"] = np.ascontiguousarray(f("moe_w_gate")[0][:ne_run])
        shared["mwu"] = np.ascontiguousarray(f("moe_w_up")[0][:ne_run])
        shared["mwd"] = np.ascontiguousarray(f("moe_w_down")[0][:ne_run])
    vecs = np.zeros((128, V_TOT), np.float32)
    for l in range(2):
        vecs[:, V_N1G + l * 16:V_N1G + (l + 1) * 16] = _pm(f("norm1_g")[l])
        vecs[:, V_N2G + l * 16:V_N2G + (l + 1) * 16] = _pm(f("norm2_g")[l])
        vecs[:, V_MODB + l * 96:V_MODB + (l + 1) * 96] = _pm(f("mod_b")[l])
    vecs[:, V_CBIN:V_CBIN + 32] = _pm(f("conv_b_in")[0])
    wdw = f("conv_w_dw")[0]
    vecs[:, V_CWDW:V_CWDW + 16 * CW] = np.ascontiguousarray(
        wdw.reshape(CW, 16, 128).transpose(2, 1, 0)).reshape(128, 16 * CW)
    vecs[:, V_CBDW:V_CBDW + 16] = _pm(f("conv_b_dw")[0])
    vecs[:, V_LNG:V_LNG + 16] = _pm(f("conv_ln_g")[0])
    vecs[:, V_LNB:V_LNB + 16] = _pm(f("conv_ln_b")[0])
    vecs[:, V_CBOUT:V_CBOUT + 16] = _pm(f("conv_b_out")[0])
    vecs[:, V_FING:V_FING + 16] = _pm(f("final_g"))
    vecs[:, V_SUBG] = f("attn_subln_g")[0]
    lam = np.concatenate([f("attn_lam_q1")[0], f("attn_lam_k1")[0], f("attn_lam_q2")[0], f("attn_lam_k2")[0]])
    vecs[:, V_LAM:V_LAM + 256] = lam[None, :]
    maps = []
    for b in cores:
        m = dict(shared)
        m["vecs"] = vecs
        m["xT"] = np.ascontiguousarray(x[b].T).reshape(KC, 128, S)
        m["cT"] = _pm(c[b])
        maps.append(m)
    return maps


def _launch(nc, maps, trace=False):
    names = set()
    for alloc in nc.allocations:
        try:
            if alloc.kind == "ExternalInput":
                names.add(alloc.memorylocations[0].name)
        except Exception:
            pass
    maps = [{k: v for k, v in m.items() if k in names} for m in maps]
    return run_bass_kernel_spmd(nc, maps, core_ids=list(range(len(maps))), trace=trace)


def run(inputs, stage=99, cores=None, trace=False, dbg=None, dbg_tile=None, ne_run=NE):
    cores = list(range(B)) if cores is None else cores
    nc = build(stage=stage, dbg=dbg, dbg_tile=dbg_tile, ne_run=ne_run)
    maps = make_in_maps(inputs, cores, stage, ne_run)
    res = _launch(nc, maps, trace)
    outs = [np.ascontiguousarray(r["outT"].reshape(D, S).T) for r in res.results]
    return np.stack(outs, axis=0), res


NE_L1 = 4


def run_multi(inputs, cores=None, trace=False):
    cores = list(range(B)) if cores is None else cores
    maps = make_in_maps(inputs, cores, stage=3, ne_run=NE_L1)
    nc1 = build(stage=3, mode="full", ne_run=NE_L1, last=False, x2out=True)
    res = _launch(nc1, maps, trace)
    x2 = [r["x2T_out"] for r in res.results]
    acc = [r["outT"] for r in res.results]
    mvec = [r["mvec_out"] for r in res.results]
    f = lambda k: np.asarray(inputs[k], np.float32)
    ne2 = NE - NE_L1
    nc2 = build(stage=3, mode="moe", e0=0, ne_run=ne2, first=False, last=True)
    consts = maps[0]["consts"].copy()
    consts[:, C_SEL:] = 0.0
    for le in range(ne2):
        consts[NE_L1 + le, C_SEL + le * 128:C_SEL + (le + 1) * 128] = 1.0
    g = np.ascontiguousarray(f("moe_w_gate")[0][NE_L1:])
    u = np.ascontiguousarray(f("moe_w_up")[0][NE_L1:])
    d = np.ascontiguousarray(f("moe_w_down")[0][NE_L1:])
    mm_ = []
    for i in range(len(cores)):
        mm_.append({"vecs": maps[i]["vecs"], "consts": consts, "wr": maps[i]["wr"], "x2T": x2[i],
                    "xaccT": acc[i], "mvec": mvec[i], "mwg": g, "mwu": u, "mwd": d})
    res = _launch(nc2, mm_, trace)
    outs = [np.ascontiguousarray(r["outT"].reshape(D, S).T) for r in res.results]
    return np.stack(outs, axis=0)


def kernel(**inputs):
    out = run_multi(inputs)
    return out.astype(np.float32)
```

```python
import math
import numpy as np
import concourse.bass as bass
import concourse.mybir as mybir
from concourse.bass_utils import run_bass_kernel_spmd

F32 = mybir.dt.float32
BF16 = mybir.dt.bfloat16
AF = mybir.ActivationFunctionType
ALU = mybir.AluOpType
AX = mybir.AxisListType

D = 2048
S = 4096
B = 8
KC = 16
TT = 512
NT = S // TT
FF = 5632
FFC = FF // 128
EF = 7168
EFC = EF // 128
NE = 8
H = 16
CW = 31
HALO = CW - 1
RMS_EPS = 1e-6
LN_EPS = 1e-5
NEG = -30000.0

V_N1G = 0
V_N2G = 32
V_MODB = 64
V_CBIN = 256
V_CWDW = 288
V_CBDW = 784
V_LNG = 800
V_LNB = 816
V_CBOUT = 832
V_FING = 848
V_SUBG = 864
V_LAM = 865
V_TOT = 1121
C_ID = 0
C_ABSD = 128
C_DMASK = 256
C_SEL = 384
C_TOT = 384 + 1024


def lam_init(layer_idx):
    return 0.8 - 0.6 * math.exp(-0.3 * layer_idx)


class Buf:
    __slots__ = ("name", "w", "r")

    def __init__(self, name):
        self.name = name
        self.w = {}
        self.r = {}


class Eng:
    def __init__(self, name, obj, sem, is_pe=False):
        self.name = name
        self.obj = obj
        self.sem = sem
        self.is_pe = is_pe
        self.count = 0
        self.pending = False
        self.waited = {}
        self.dma_sems = []
        self.dma_uses = []
        self.dma_next = 0


class Tracker:
    def __init__(self, nc, n_dma_sems=10):
        self.nc = nc
        self.sems = []

        def newsem(name):
            s = nc.alloc_semaphore(name)
            self.sems.append(s)
            return len(self.sems) - 1

        self.eng = {
            "pe": Eng("pe", nc.tensor, newsem("s_pe"), is_pe=True),
            "act": Eng("act", nc.scalar, newsem("s_act")),
            "dve": Eng("dve", nc.vector, newsem("s_dve")),
            "pool": Eng("pool", nc.gpsimd, newsem("s_pool")),
            "sp": Eng("sp", nc.sync, newsem("s_sp")),
        }
        for q in ("sp", "pool", "act"):
            e = self.eng[q]
            for i in range(n_dma_sems):
                e.dma_sems.append(newsem(f"d_{q}{i}"))
                e.dma_uses.append(0)

    def _wait(self, e, need):
        for s, v in need.items():
            if e.waited.get(s, 0) >= v:
                continue
            e.obj.wait_ge(self.sems[s], v)
            e.waited[s] = v

    def _need(self, e, r, w, nowaw=False):
        need = {}

        def add(d, allow_same):
            for s, v in d.items():
                if s == e.sem and not allow_same:
                    continue
                if need.get(s, 0) < v:
                    need[s] = v

        for b in r:
            add(b.w, not e.is_pe)
        for b in w:
            if not nowaw:
                add(b.w, not e.is_pe)
            add(b.r, False)
        return need

    def op(self, en, fn, r=(), w=(), inc=True):
        e = self.eng[en]
        need = self._need(e, r, w)
        if e.pending:
            assert need.get(e.sem, 0) <= e.count, "self-dep on pending token"
        self._wait(e, need)
        ins = fn()
        tok = e.count + 1
        if inc:
            ins.then_inc(self.sems[e.sem], 1)
            e.count = tok
            e.pending = False
        else:
            assert e.is_pe
            e.pending = True
        for b in r:
            if b.r.get(e.sem, 0) < tok:
                b.r[e.sem] = tok
        for b in w:
            b.w = {e.sem: tok}
            b.r = {}
        return ins

    def dma(self, q, out, in_, r=(), w=(), accum=False, **kw):
        e = self.eng[q]
        k = e.dma_next
        e.dma_next = (k + 1) % len(e.dma_sems)
        s = e.dma_sems[k]
        uses = e.dma_uses[k]
        need = self._need(e, r, w, nowaw=accum)
        if uses > 0 and need.get(s, 0) < 16 * uses:
            need[s] = 16 * uses
        self._wait(e, need)
        ins = e.obj.dma_start(out=out, in_=in_, **kw)
        ins.then_inc(self.sems[s], 16)
        e.dma_uses[k] = uses + 1
        tok = 16 * (uses + 1)
        for b in r:
            if b.r.get(s, 0) < tok:
                b.r[s] = tok
        for b in w:
            if accum:
                if b.w.get(s, 0) < tok:
                    b.w[s] = tok
            else:
                b.w = {s: tok}
                b.r = {}
        return ins

    def barrier(self, full=False):
        need = {}
        for e in self.eng.values():
            assert not e.pending
            if e.count > 0:
                need[e.sem] = e.count
            if e.name == "pool" and not full:
                continue
            for s, u in zip(e.dma_sems, e.dma_uses):
                if u > 0:
                    need[s] = 16 * u
        for e in self.eng.values():
            n2 = {s: v for s, v in need.items() if s != e.sem}
            self._wait(e, n2)


class Tile:
    def __init__(self, t, name):
        self.t = t
        self.b = Buf(name)

    def __getitem__(self, idx):
        return self.t[idx]


def build(stage=99, dbg=None, dbg_tile=None, ne_run=NE, mode="full", e0=0, first=True, last=True, x2out=False):
    nc = bass.Bass("TRN2", target_bir_lowering=False)
    T = Tracker(nc)

    def din(name, shape, dt=F32):
        return nc.dram_tensor(name, list(shape), dt, kind="ExternalInput").ap()

    def dscr(name, shape, dt):
        return Tile(nc.dram_tensor(name, list(shape), dt, kind="Internal").ap(), name)

    front = mode in ("full", "front")
    vecs_in = din("vecs", [128, V_TOT])
    consts_in = din("consts", [128, C_TOT])
    wr_in = din("wr", [D, NE])
    w_in = {}
    if front:
        xT_in = din("xT", [KC, 128, S])
        cT_in = din("cT", [128, KC])
        base_in = din("base", [128, S])
        mod_w = din("mod_w", [2, D, 6 * D])
        w_in = {
            "cwin": din("cwin", [D, 2 * D]),
            "cwout": din("cwout", [D, D]),
            "fwg": din("fwg", [D, FF]),
            "fwu": din("fwu", [D, FF]),
            "fwd": din("fwd", [FF, D]),
            "wqkv": din("wqkv", [D, 3 * D]),
            "wo": din("wo", [D, D]),
        }
    else:
        x2T_in = din("x2T", [KC, 128, S])
        mvec_in = din("mvec", [128, 368])
        if not first:
            xacc_in = din("xaccT", [KC, 128, S])
    if x2out:
        x2T_out = Tile(nc.dram_tensor("x2T_out", [KC, 128, S], F32, kind="ExternalOutput").ap(), "x2T_out")
    if mode == "front" or x2out:
        mvec_out = Tile(nc.dram_tensor("mvec_out", [128, 368], F32, kind="ExternalOutput").ap(), "mvec_out")
    if stage >= 3 and mode != "front":
        mwg_in = din("mwg", [ne_run, D, EF])
        mwu_in = din("mwu", [ne_run, D, EF])
        mwd_in = din("mwd", [ne_run, EF, D])
    outT = Tile(nc.dram_tensor("outT", [KC, 128, S], F32, kind="ExternalOutput").ap(), "outT")

    wb = {}
    for k, a in w_in.items():
        wb[k] = dscr("b_" + k, a.shape, BF16)
    mwg_b = [dscr(f"b_mwg{e}", [D, EF], BF16) for e in range(ne_run)]
    mwu_b = [dscr(f"b_mwu{e}", [D, EF], BF16) for e in range(ne_run)]
    mwd_b = [dscr(f"b_mwd{e}", [EF, D], BF16) for e in range(ne_run)]
    x1T = dscr("x1T", [KC, 128, S], F32)
    qT_s = dscr("qT_s", [H, 128, S], BF16)
    kT_s = dscr("kT_s", [H, 128, S], BF16)
    v_s = dscr("v_s", [H, 128, S // 128, 128], BF16)

    off = {"p": 16512}
    uid = {"n": 0}

    def _nbytes(shape, dt):
        n = 1
        for s_ in shape[1:]:
            n *= s_
        return n * (2 if dt == BF16 else 4)

    def sbp(name, shape, dt):
        t = nc.alloc_sbuf_tensor_at(name, list(shape), dt, offset=off["p"])
        off["p"] += (_nbytes(shape, dt) + 31) // 32 * 32
        return Tile(t, name)

    def ps_alloc(name):
        return Tile(nc.alloc_psum_tensor(name, [128, 512], F32), name)

    vecs = sbp("vecs", [128, V_TOT], F32)
    consts = sbp("consts", [128, C_TOT], F32)
    modT = sbp("modT", [128, 2 * 96], F32)
    drv = sbp("drv", [128, 176], F32)
    cvec = sbp("cvec", [128, 64], F32)
    ones_bf = sbp("ones_bf", [128, 128], BF16)
    wr_sb = sbp("wr_sb", [128, KC, NE], F32)
    xs = sbp("xs", [128, KC, TT], F32)
    hT = sbp("hT", [128, KC, TT], BF16)
    NSLAB = 4
    slabs = [sbp(f"slab{i}", [128, KC, 512], BF16) for i in range(NSLAB)]
    NTMP = 5
    tmps = [sbp(f"tmp{i}", [128, 512], F32) for i in range(NTMP)]
    NLT = 3
    ltmps = [sbp(f"ltmp{i}", [128, 512], F32) for i in range(NLT)]
    R0 = off["p"]
    RCAP = 229376 - R0
    assert RCAP >= 66496, RCAP

    def sbr(name, shape, dt, o, buf=None):
        assert o + _nbytes(shape, dt) <= RCAP, (name, o, _nbytes(shape, dt), RCAP)
        uid["n"] += 1
        t = nc.alloc_sbuf_tensor_at(f"{name}_{uid['n']}", list(shape), dt, offset=R0 + o)
        tl = Tile(t, name)
        if buf is not None:
            tl.b = buf
        return tl

    psb = [ps_alloc(f"ps{i}") for i in range(8)]
    st = {"slab": 0, "tmp": 0, "ps": 0, "lt": 0}

    def next_slab():
        s = slabs[st["slab"] % NSLAB]
        st["slab"] += 1
        return s

    def tmp():
        s = tmps[st["tmp"] % NTMP]
        st["tmp"] += 1
        return s

    def ltmp():
        s = ltmps[st["lt"] % NLT]
        st["lt"] += 1
        return s

    def ps(lo=0, hi=8):
        n = hi - lo
        s = psb[lo + st["ps"] % n]
        st["ps"] += 1
        return s

    def V(col, n=1):
        return vecs[:, col:col + n]

    def mm(out_ap, lhsT, rhs, start, stop, r, w, inc):
        return T.op("pe", lambda: nc.tensor.matmul(out_ap, lhsT=lhsT, rhs=rhs, start=start, stop=stop),
                    r=r, w=w, inc=inc)

    def load_slab(wt, kc0, nkc, c0, ncols=512):
        s = next_slab()
        src = wt.t.rearrange("(kc p) n -> p kc n", p=128)[:, kc0:kc0 + nkc, c0:c0 + ncols]
        T.dma("sp", s[:, 0:nkc, 0:ncols], src, r=[wt.b], w=[s.b])
        return s

    def xview(tl):
        return tl.rearrange("c p t -> p c t")

    def precast(src_ap, dst, K, N):
        RBK = 128
        for i in range(K // RBK):
            o = dst.t[i * RBK:(i + 1) * RBK, :].rearrange("k (a b) -> k a b", b=512)
            ii = src_ap[i * RBK:(i + 1) * RBK, :].rearrange("k (a b) -> k a b", b=512)
            T.dma("pool", o, ii, w=[dst.b], accum=True)

    T.dma("sp", vecs[:, :], vecs_in[:, :], w=[vecs.b])
    T.dma("sp", consts[:, :], consts_in[:, :], w=[consts.b])
    if front:
        T.dma("sp", cvec[:, 0:KC], cT_in[:, :], w=[cvec.b])
    else:
        T.dma("sp", modT[:, :], mvec_in[:, 0:192], w=[modT.b])
        T.dma("sp", drv[:, :], mvec_in[:, 192:368], w=[drv.b])
    T.dma("sp", wr_sb[:, :, :], wr_in.rearrange("(kc p) e -> p kc e", p=128), w=[wr_sb.b])
    T.op("dve", lambda: nc.vector.memset(ones_bf[:, :], 1.0), w=[ones_bf.b])

    for k in ("cwin", "cwout", "fwg", "fwu", "fwd", "wqkv", "wo"):
        if front and (stage >= 2 or k in ("cwin", "cwout", "fwg", "fwu", "fwd")):
            precast(w_in[k], wb[k], w_in[k].shape[0], w_in[k].shape[1])
    if stage >= 3 and mode != "front":
        for e in range(ne_run):
            precast(mwg_in[e], mwg_b[e], D, EF)
            precast(mwu_in[e], mwu_b[e], D, EF)
            precast(mwd_in[e], mwd_b[e], EF, D)

    def phase0():
        T.op("act", lambda: nc.scalar.activation(out=cvec[:, 16:32], in_=cvec[:, 0:16], func=AF.Silu),
             r=[cvec.b], w=[cvec.b])
        MSL = 512
        mws = [sbr("mw0", [128, KC, MSL], F32, 0), sbr("mw1", [128, KC, MSL], F32, 32768)]
        cnt = 0
        for l in range(2):
            pm = ps()
            for sgi in range(6 * D // MSL):
                mwt = mws[cnt % 2]
                cnt += 1
                src = mod_w[l].rearrange("(kc p) n -> p kc n", p=128)[:, :, sgi * MSL:(sgi + 1) * MSL]
                T.dma("sp", mwt[:, :, :], src, w=[mwt.b])
                for j4 in range(MSL // 128):
                    j = sgi * (MSL // 128) + j4
                    for kc in range(KC):
                        mm(pm[:, j:j + 1], mwt[:, kc, j4 * 128:(j4 + 1) * 128], cvec[:, 16 + kc:17 + kc],
                           kc == 0, kc == KC - 1, r=[mwt.b, cvec.b], w=[pm.b],
                           inc=(kc == KC - 1 and j4 == MSL // 128 - 1))
            T.op("dve", lambda: nc.vector.tensor_tensor(out=modT[:, l * 96:(l + 1) * 96], in0=pm[:, 0:96],
                                                        in1=V(V_MODB + l * 96, 96), op=ALU.add),
                 r=[pm.b, vecs.b], w=[modT.b])
        for l in range(2):
            o = l * 80
            m = l * 96
            T.op("dve", lambda: nc.vector.scalar_tensor_tensor(
                out=drv[:, o:o + 16], in0=modT[:, m + 16:m + 32], scalar=1.0, in1=V(V_N1G + l * 16, 16),
                op0=ALU.add, op1=ALU.mult), r=[modT.b, vecs.b], w=[drv.b])
            T.op("dve", lambda: nc.vector.scalar_tensor_tensor(
                out=drv[:, o + 16:o + 32], in0=modT[:, m + 64:m + 80], scalar=1.0, in1=V(V_N2G + l * 16, 16),
                op0=ALU.add, op1=ALU.mult), r=[modT.b, vecs.b], w=[drv.b])
        T.op("dve", lambda: nc.vector.tensor_tensor(out=drv[:, 32:48], in0=modT[:, 32:48], in1=V(V_CBOUT, 16),
                                                    op=ALU.mult), r=[modT.b, vecs.b], w=[drv.b])
        t0 = tmp()
        T.op("dve", lambda: nc.vector.tensor_tensor(out=t0[:, 0:64], in0=V(V_LAM, 64), in1=V(V_LAM + 64, 64),
                                                    op=ALU.mult), r=[vecs.b], w=[t0.b])
        T.op("dve", lambda: nc.vector.tensor_tensor(out=t0[:, 64:128], in0=V(V_LAM + 128, 64), in1=V(V_LAM + 192, 64),
                                                    op=ALU.mult), r=[vecs.b], w=[t0.b])
        T.op("dve", lambda: nc.vector.reduce_sum(out=drv[:, 162:163], in_=t0[:, 0:64], axis=AX.X), r=[t0.b], w=[drv.b])
        T.op("dve", lambda: nc.vector.reduce_sum(out=drv[:, 163:164], in_=t0[:, 64:128], axis=AX.X), r=[t0.b], w=[drv.b])
        T.op("act", lambda: nc.scalar.activation(out=drv[:, 164:166], in_=drv[:, 162:164], func=AF.Exp),
             r=[drv.b], w=[drv.b])
        T.op("dve", lambda: nc.vector.scalar_tensor_tensor(
            out=drv[:, 160:161], in0=drv[:, 164:165], scalar=lam_init(1), in1=drv[:, 165:166],
            op0=ALU.add, op1=ALU.subtract), r=[drv.b], w=[drv.b])
        T.op("dve", lambda: nc.vector.tensor_scalar(out=drv[:, 161:162], in0=drv[:, 160:161], scalar1=-1.0, scalar2=None,
                                                    op0=ALU.mult), r=[drv.b], w=[drv.b])
        T.op("dve", lambda: nc.vector.tensor_scalar(out=drv[:, 166:167], in0=V(V_SUBG, 1), scalar1=1.0 - lam_init(1),
                                                    scalar2=None, op0=ALU.mult), r=[vecs.b], w=[drv.b])
        T.barrier()


    if front:
        phase0()
    else:
        T.barrier()

    def MOD(l, which):
        m = l * 96 + which * 16
        return lambda c: modT[:, m + c:m + c + 1]

    def DRV(col):
        return lambda c: drv[:, col + c:col + c + 1]

    cur = {}

    def rms_stats(src3):
        sq = cur["sq"]
        T.op("act", lambda: nc.scalar.activation(out=sq[:, :, :], in_=src3[:, :, :], func=AF.Square),
             r=[src3.b], w=[sq.b])
        p = ps()
        for kc in range(KC):
            mm(p[:, :], ones_bf[:, :], sq[:, kc, :], kc == 0, kc == KC - 1, r=[sq.b, ones_bf.b], w=[p.b],
               inc=(kc == KC - 1))
        rstd = ltmp()
        T.op("act", lambda: nc.scalar.activation(out=rstd[:, :], in_=p[:, :], func=AF.Sqrt, bias=RMS_EPS,
                                                 scale=1.0 / D), r=[p.b], w=[rstd.b])
        T.op("dve", lambda: nc.vector.reciprocal(out=rstd[:, :], in_=rstd[:, :]), r=[rstd.b], w=[rstd.b])
        return rstd

    def rms_mod(A, Bsh, post_chunk=None):
        rstd = rms_stats(xs)
        for c in range(KC):
            t = tmp()
            T.op("dve", lambda: nc.vector.scalar_tensor_tensor(
                out=t[:, :], in0=xs[:, c, :], scalar=A(c), in1=rstd[:, :], op0=ALU.mult, op1=ALU.mult),
                r=[xs.b, rstd.b, drv.b, modT.b], w=[t.b])
            if post_chunk is None:
                T.op("act", lambda: nc.scalar.activation(out=hT[:, c, :], in_=t[:, :], func=AF.Identity,
                                                         bias=Bsh(c), scale=1.0),
                     r=[t.b, modT.b], w=[hT.b])
            else:
                T.op("dve", lambda: nc.vector.tensor_scalar(out=t[:, :], in0=t[:, :], scalar1=Bsh(c), scalar2=None,
                                                            op0=ALU.add), r=[t.b, modT.b], w=[t.b])
                T.op("act", lambda: nc.scalar.activation(out=hT[:, c, :], in_=t[:, :], func=AF.Copy),
                     r=[t.b], w=[hT.b])
                post_chunk(c, t)

    def down_proj(wt, nkc, actb, G, kgroup=KC):
        kgs = []
        k0 = 0
        while k0 < nkc:
            kgs.append((k0, min(kgroup, nkc - k0)))
            k0 += kgroup
        for cg in range(4):
            pbank = [ps(0, 4) for _ in range(4)]
            for gi, (k0, nk) in enumerate(kgs):
                s = load_slab(wt, k0, nk, cg * 512)
                for j4 in range(4):
                    p = pbank[j4]
                    for kk in range(nk):
                        first = (gi == 0 and kk == 0)
                        last = (gi == len(kgs) - 1 and kk == nk - 1)
                        mm(p[:, :], s[:, kk, j4 * 128:(j4 + 1) * 128], actb[:, k0 + kk, :], first, last,
                           r=[s.b, actb.b], w=[p.b], inc=(kk == nk - 1))
            for j4 in range(4):
                mc = cg * 4 + j4
                p = pbank[j4]
                T.op("dve", lambda: nc.vector.scalar_tensor_tensor(
                    out=xs[:, mc, :], in0=p[:, :], scalar=G(mc), in1=xs[:, mc, :], op0=ALU.mult, op1=ALU.add),
                    r=[p.b, xs.b, modT.b], w=[xs.b])

    def gate_up(wg, wu, nslab, actb, cb=None):
        for sgi in range(nslab):
            s1 = load_slab(wg, 0, KC, sgi * 512)
            s2 = load_slab(wu, 0, KC, sgi * 512)
            for j4 in range(4):
                j = sgi * 4 + j4
                pg = ps(4, 8)
                pu = ps(4, 8)
                for kc in range(KC):
                    mm(pg[:, :], s1[:, kc, j4 * 128:(j4 + 1) * 128], hT[:, kc, :], kc == 0, kc == KC - 1,
                       r=[s1.b, hT.b], w=[pg.b], inc=(kc == KC - 1))
                for kc in range(KC):
                    mm(pu[:, :], s2[:, kc, j4 * 128:(j4 + 1) * 128], hT[:, kc, :], kc == 0, kc == KC - 1,
                       r=[s2.b, hT.b], w=[pu.b], inc=(kc == KC - 1))
                sg_ = tmp()
                T.op("act", lambda: nc.scalar.activation(out=sg_[:, :], in_=pg[:, :], func=AF.Silu),
                     r=[pg.b], w=[sg_.b])
                if cb is not None:
                    T.op("dve", lambda: nc.vector.tensor_tensor(out=sg_[:, :], in0=sg_[:, :], in1=cb[:, :],
                                                                op=ALU.mult), r=[sg_.b, cb.b], w=[sg_.b])
                T.op("dve", lambda: nc.vector.tensor_tensor(out=actb[:, j, :], in0=pu[:, :], in1=sg_[:, :],
                                                            op=ALU.mult), r=[pu.b, sg_.b], w=[actb.b])


    class DbgStop(Exception):
        pass

    def dump(src3, buf, n=KC, tile=0):
        T.op("act", lambda: nc.scalar.activation(out=xs[:, 0:n, :], in_=src3, func=AF.Copy), r=[buf], w=[xs.b])
        T.dma("sp", xview(outT.t)[:, :, tile * TT:(tile + 1) * TT], xs[:, :, :], r=[xs.b], w=[outT.b], accum=True)
        T.barrier()
        raise DbgStop()

    def phase_ab():
        ub = sbr("ub", [128, KC, HALO + TT], BF16, 0)
        UO = 17344
        ubuf = Buf("U")
        vb = sbr("vb", [128, KC, TT], F32, UO, ubuf)
        sq = sbr("sq", [128, KC, TT], BF16, UO + 32768, ubuf)
        actb = sbr("actb", [128, FFC, TT], BF16, UO, ubuf)
        cur["sq"] = sq
        hsave = sbr("hsave", [128, KC, HALO], F32, UO + 49152)
        T.op("dve", lambda: nc.vector.memset(ub[:, :, 0:HALO], 0.0), w=[ub.b])
        for i in range(NT):
            T.dma("sp", xs[:, :, :], xview(xT_in)[:, :, i * TT:(i + 1) * TT], w=[xs.b])
            rms_mod(DRV(0), MOD(0, 0))
            if dbg == 'mod':
                T.op('act', lambda: nc.scalar.activation(out=xs[:, 0, 0:192], in_=modT[:, :], func=AF.Copy), r=[modT.b], w=[xs.b])
                T.op('act', lambda: nc.scalar.activation(out=xs[:, 1, 0:176], in_=drv[:, :], func=AF.Copy), r=[drv.b], w=[xs.b])
                dump(xs[:, :, :], xs.b)
            if dbg == 'h1':
                dump(hT[:, :, :], hT.b)
            for sgi in range(4):
                sv = load_slab(wb["cwin"], 0, KC, sgi * 512)
                sg = load_slab(wb["cwin"], 0, KC, D + sgi * 512)
                for j4 in range(4):
                    j = sgi * 4 + j4
                    pv = ps()
                    pg = ps()
                    for kc in range(KC):
                        mm(pv[:, :], sv[:, kc, j4 * 128:(j4 + 1) * 128], hT[:, kc, :], kc == 0, kc == KC - 1,
                           r=[sv.b, hT.b], w=[pv.b], inc=(kc == KC - 1))
                    for kc in range(KC):
                        mm(pg[:, :], sg[:, kc, j4 * 128:(j4 + 1) * 128], hT[:, kc, :], kc == 0, kc == KC - 1,
                           r=[sg.b, hT.b], w=[pg.b], inc=(kc == KC - 1))
                    sgm = tmp()
                    T.op("act", lambda: nc.scalar.activation(out=sgm[:, :], in_=pg[:, :], func=AF.Sigmoid,
                                                             bias=V(V_CBIN + 16 + j), scale=1.0),
                         r=[pg.b, vecs.b], w=[sgm.b])
                    T.op("dve", lambda: nc.vector.scalar_tensor_tensor(
                        out=ub[:, j, HALO:HALO + TT], in0=pv[:, :], scalar=V(V_CBIN + j), in1=sgm[:, :],
                        op0=ALU.add, op1=ALU.mult), r=[pv.b, sgm.b, vecs.b], w=[ub.b])
            if dbg == 'u':
                dump(ub[:, :, HALO:HALO + TT], ub.b)
            for j in range(KC):
                for wtap in range(CW):
                    wcol = V(V_CWDW + j * CW + wtap)
                    if wtap == 0:
                        T.op("dve", lambda: nc.vector.tensor_scalar(
                            out=vb[:, j, :], in0=ub[:, j, 0:TT], scalar1=wcol, scalar2=V(V_CBDW + j),
                            op0=ALU.mult, op1=ALU.add), r=[ub.b, vecs.b], w=[vb.b])
                    else:
                        T.op("dve", lambda: nc.vector.scalar_tensor_tensor(
                            out=vb[:, j, :], in0=ub[:, j, wtap:wtap + TT], scalar=wcol, in1=vb[:, j, :],
                            op0=ALU.mult, op1=ALU.add), r=[ub.b, vecs.b], w=[vb.b])
            if dbg == 'v':
                dump(vb[:, :, :], vb.b)
            hs = hsave
            hsv = hs[:, :, :]
            T.op("act", lambda: nc.scalar.activation(out=hsv, in_=ub[:, :, TT:TT + HALO], func=AF.Copy),
                 r=[ub.b], w=[hs.b])
            zq = hT
            T.op("act", lambda: nc.scalar.activation(out=zq[:, :, :], in_=vb[:, :, :], func=AF.Copy),
                 r=[vb.b], w=[zq.b])
            T.op("act", lambda: nc.scalar.activation(out=sq[:, :, :], in_=vb[:, :, :], func=AF.Square),
                 r=[vb.b], w=[sq.b])
            p1 = ps()
            p2 = ps()
            for kc in range(KC):
                mm(p1[:, :], ones_bf[:, :], zq[:, kc, :], kc == 0, kc == KC - 1, r=[zq.b, ones_bf.b], w=[p1.b],
                   inc=(kc == KC - 1))
            for kc in range(KC):
                mm(p2[:, :], ones_bf[:, :], sq[:, kc, :], kc == 0, kc == KC - 1, r=[sq.b, ones_bf.b], w=[p2.b],
                   inc=(kc == KC - 1))
            mean = ltmp()
            rstd = ltmp()
            nmr = ltmp()
            T.op("dve", lambda: nc.vector.tensor_scalar(out=mean[:, :], in0=p1[:, :], scalar1=1.0 / D, scalar2=None,
                                                        op0=ALU.mult), r=[p1.b], w=[mean.b])
            T.op("dve", lambda: nc.vector.tensor_tensor(out=nmr[:, :], in0=mean[:, :], in1=mean[:, :], op=ALU.mult),
                 r=[mean.b], w=[nmr.b])
            T.op("dve", lambda: nc.vector.scalar_tensor_tensor(
                out=rstd[:, :], in0=p2[:, :], scalar=1.0 / D, in1=nmr[:, :], op0=ALU.mult, op1=ALU.subtract),
                r=[p2.b, nmr.b], w=[rstd.b])
            T.op("act", lambda: nc.scalar.activation(out=rstd[:, :], in_=rstd[:, :], func=AF.Sqrt, bias=LN_EPS,
                                                     scale=1.0), r=[rstd.b], w=[rstd.b])
            T.op("dve", lambda: nc.vector.reciprocal(out=rstd[:, :], in_=rstd[:, :]), r=[rstd.b], w=[rstd.b])
            T.op("dve", lambda: nc.vector.scalar_tensor_tensor(
                out=nmr[:, :], in0=mean[:, :], scalar=-1.0, in1=rstd[:, :], op0=ALU.mult, op1=ALU.mult),
                r=[mean.b, rstd.b], w=[nmr.b])
            for j in range(KC):
                t = tmp()
                T.op("dve", lambda: nc.vector.tensor_tensor(out=t[:, :], in0=vb[:, j, :], in1=rstd[:, :], op=ALU.mult),
                     r=[vb.b, rstd.b], w=[t.b])
                T.op("dve", lambda: nc.vector.tensor_tensor(out=t[:, :], in0=t[:, :], in1=nmr[:, :], op=ALU.add),
                     r=[t.b, nmr.b], w=[t.b])
                T.op("act", lambda: nc.scalar.activation(out=ub[:, j, HALO:HALO + TT], in_=t[:, :], func=AF.Silu,
                                                         bias=V(V_LNB + j), scale=V(V_LNG + j)),
                     r=[t.b, vecs.b], w=[ub.b])
            if dbg == 'z':
                dump(ub[:, :, HALO:HALO + TT], ub.b)
            for sgi in range(4):
                so = load_slab(wb["cwout"], 0, KC, sgi * 512)
                for j4 in range(4):
                    mc = sgi * 4 + j4
                    p = ps()
                    for kc in range(KC):
                        mm(p[:, :], so[:, kc, j4 * 128:(j4 + 1) * 128], ub[:, kc, HALO:HALO + TT], kc == 0,
                           kc == KC - 1, r=[so.b, ub.b], w=[p.b], inc=(kc == KC - 1))
                    t = tmp()
                    T.op("act", lambda: nc.scalar.activation(out=t[:, :], in_=p[:, :], func=AF.Identity,
                                                             bias=drv[:, 32 + mc:33 + mc],
                                                             scale=modT[:, 32 + mc:33 + mc]),
                         r=[p.b, drv.b, modT.b], w=[t.b])
                    T.op("dve", lambda: nc.vector.tensor_tensor(out=xs[:, mc, :], in0=xs[:, mc, :], in1=t[:, :],
                                                                op=ALU.add), r=[t.b, xs.b], w=[xs.b])
            if dbg == 'xa':
                dump(xs[:, :, :], xs.b)
            T.op("act", lambda: nc.scalar.activation(out=ub[:, :, 0:HALO], in_=hsv, func=AF.Copy),
                 r=[hs.b], w=[ub.b])
            rms_mod(DRV(16), MOD(0, 3))
            gate_up(wb["fwg"], wb["fwu"], FF // 512, actb)
            down_proj(wb["fwd"], FFC, actb, MOD(0, 5))
            T.dma("act", xview(x1T.t)[:, :, i * TT:(i + 1) * TT], xs[:, :, :], r=[xs.b], w=[x1T.b], accum=True)
        T.barrier()

    try:
        if front:
            phase_ab()
    except DbgStop:
        return nc
    if stage <= 1:
        for i in range(NT):
            T.dma("sp", xs[:, :, :], xview(x1T.t)[:, :, i * TT:(i + 1) * TT], r=[x1T.b], w=[xs.b])
            T.dma("sp", xview(outT.t)[:, :, i * TT:(i + 1) * TT], xs[:, :, :], r=[xs.b], w=[outT.b], accum=True)
        T.barrier()
        return nc

    def phase_c1():
        sq = sbr("sq1", [128, KC, TT], BF16, 0)
        cur["sq"] = sq
        qkb = [sbr(f"qkb{i}", [128, 4, TT], BF16, 16384 + i * 4096) for i in range(2)]
        cntq = 0
        for i in range(NT):
            T.dma("sp", xs[:, :, :], xview(x1T.t)[:, :, i * TT:(i + 1) * TT], r=[x1T.b], w=[xs.b])
            rms_mod(DRV(80), MOD(1, 0))
            for sgi in range(12):
                s = load_slab(wb["wqkv"], 0, KC, sgi * 512)
                ob = qkb[cntq % 2]
                cntq += 1
                if sgi < 8:
                    dst = qT_s if sgi < 4 else kT_s
                    scale = 0.125 if sgi < 4 else 1.0
                    for j4 in range(4):
                        p = ps()
                        for kc in range(KC):
                            mm(p[:, :], s[:, kc, j4 * 128:(j4 + 1) * 128], hT[:, kc, :], kc == 0, kc == KC - 1,
                               r=[s.b, hT.b], w=[p.b], inc=(kc == KC - 1))
                        if j4 % 2 == 0:
                            T.op("act", lambda: nc.scalar.activation(out=ob[:, j4, :], in_=p[:, :], func=AF.Copy,
                                                                     scale=scale), r=[p.b], w=[ob.b])
                        else:
                            T.op("dve", lambda: nc.vector.tensor_scalar(out=ob[:, j4, :], in0=p[:, :], scalar1=scale,
                                                                        scalar2=None, op0=ALU.mult),
                                 r=[p.b], w=[ob.b])
                    h0 = (sgi % 4) * 4
                    T.dma("act", dst.t[h0:h0 + 4, :, i * TT:(i + 1) * TT].rearrange("h p t -> p h t"), ob[:, :, :],
                          r=[ob.b], w=[dst.b], accum=True)
                else:
                    h0 = (sgi - 8) * 4
                    for tb in range(4):
                        p = ps()
                        for kc in range(KC):
                            mm(p[:, :], hT[:, kc, tb * 128:(tb + 1) * 128], s[:, kc, :], kc == 0, kc == KC - 1,
                               r=[s.b, hT.b], w=[p.b], inc=(kc == KC - 1))
                        if tb % 2 == 0:
                            T.op("act", lambda: nc.scalar.activation(out=ob[:, tb, :], in_=p[:, :], func=AF.Copy),
                                 r=[p.b], w=[ob.b])
                        else:
                            T.op("dve", lambda: nc.vector.tensor_copy(out=ob[:, tb, :], in_=p[:, :]),
                                 r=[p.b], w=[ob.b])
                    for hh in range(4):
                        T.dma("act", v_s.t[h0 + hh, :, i * 4:(i + 1) * 4, :], ob[:, :, hh * 128:(hh + 1) * 128],
                              r=[ob.b], w=[v_s.b], accum=True)
        T.barrier()

    def attention_tile(qi):
        q0 = qi * TT
        nkb = 4 * (qi + 1)
        klen = (qi + 1) * TT
        qb = [sbr(f"qb{i}", [128, TT], BF16, i * 1024) for i in range(2)]
        kbf = [sbr(f"kb{i}", [128, S], BF16, 2048 + i * 8192) for i in range(2)]
        vbf = [sbr(f"vb{i}", [128, S // 128, 128], BF16, 18432 + i * 8192) for i in range(2)]
        base = sbr("base", [128, S], F32, 34816)
        diag = sbr("diag", [128, H, 128], F32, 51200)
        pts = [sbr(f"pt{i}", [128, TT], BF16, 59392 + i * 1024) for i in range(4)]
        osq = sbr("osq", [128, TT], BF16, 63488)
        T.dma("sp", base[:, :], base_in[:, :], w=[base.b])
        T.dma("sp", xs[:, :, :], xview(x1T.t)[:, :, q0:q0 + TT], r=[x1T.b], w=[xs.b])
        for h in range(H):
            slope = 2.0 ** (-0.5 * (h + 1))
            T.op("dve", lambda: nc.vector.scalar_tensor_tensor(
                out=diag[:, h, :], in0=consts[:, C_ABSD:C_ABSD + 128], scalar=-slope,
                in1=consts[:, C_DMASK:C_DMASK + 128], op0=ALU.mult, op1=ALU.add), r=[consts.b], w=[diag.b])

        def load_head(h):
            T.dma("sp", qb[h % 2][:, :], qT_s.t[h, :, q0:q0 + TT], r=[qT_s.b], w=[qb[h % 2].b])
            T.dma("sp", kbf[h % 2][:, 0:klen], kT_s.t[h, :, 0:klen], r=[kT_s.b], w=[kbf[h % 2].b])
            T.dma("sp", vbf[h % 2][:, 0:nkb, :], v_s.t[h, :, 0:nkb, :], r=[v_s.b], w=[vbf[h % 2].b])

        load_head(0)
        ptc = 0
        U1, U2, Z1, Z2 = psb[0], psb[1], psb[2], psb[3]
        for h in range(H):
            if h + 1 < H:
                load_head(h + 1)
            slope = 2.0 ** (-0.5 * (h + 1))
            q_, k_, v_ = qb[h % 2], kbf[h % 2], vbf[h % 2]
            def scores(kb):
                o = kb - 4 * qi
                c0 = max(0, o) * 128
                s1 = ps(4, 8)
                s2 = ps(4, 8)
                mm(s1[:, c0:TT], k_[0:64, kb * 128:(kb + 1) * 128], q_[0:64, c0:TT], True, True,
                   r=[k_.b, q_.b], w=[s1.b], inc=True)
                mm(s2[:, c0:TT], k_[64:128, kb * 128:(kb + 1) * 128], q_[64:128, c0:TT], True, True,
                   r=[k_.b, q_.b], w=[s2.b], inc=True)
                return (s1, s2, c0, o)

            sc = scores(0)
            for kb in range(nkb):
                s1, s2, c0, o = sc
                sc = scores(kb + 1) if kb + 1 < nkb else None
                cur_pts = []
                for sx in (s1, s2):
                    t = tmp()
                    if o >= 0:
                        T.op("dve", lambda: nc.vector.tensor_tensor(out=t[:, c0:c0 + 128], in0=sx[:, c0:c0 + 128],
                                                                    in1=diag[:, h, :], op=ALU.add),
                             r=[sx.b, diag.b], w=[t.b])
                        if c0 + 128 < TT:
                            n = TT - c0 - 128
                            T.op("dve", lambda: nc.vector.scalar_tensor_tensor(
                                out=t[:, c0 + 128:TT], in0=base[:, 128:128 + n], scalar=-slope,
                                in1=sx[:, c0 + 128:TT], op0=ALU.mult, op1=ALU.add), r=[sx.b, base.b], w=[t.b])
                    else:
                        x0 = (4 * qi - kb) * 128
                        T.op("dve", lambda: nc.vector.scalar_tensor_tensor(
                            out=t[:, :], in0=base[:, x0:x0 + TT], scalar=-slope, in1=sx[:, :],
                            op0=ALU.mult, op1=ALU.add), r=[sx.b, base.b], w=[t.b])
                    pt = pts[ptc % 4]
                    ptc += 1
                    T.op("act", lambda: nc.scalar.activation(out=pt[:, c0:TT], in_=t[:, c0:TT], func=AF.Exp),
                         r=[t.b], w=[pt.b])
                    cur_pts.append(pt)
                first_ = kb == 0
                last_ = kb == nkb - 1
                mm(U1[:, c0:TT], v_[:, kb, :], cur_pts[0][:, c0:TT], first_, last_, r=[v_.b, cur_pts[0].b],
                   w=[U1.b], inc=last_)
                mm(Z1[:, c0:TT], ones_bf[:, :], cur_pts[0][:, c0:TT], first_, last_, r=[ones_bf.b, cur_pts[0].b],
                   w=[Z1.b], inc=last_)
                mm(U2[:, c0:TT], v_[:, kb, :], cur_pts[1][:, c0:TT], first_, last_, r=[v_.b, cur_pts[1].b],
                   w=[U2.b], inc=last_)
                mm(Z2[:, c0:TT], ones_bf[:, :], cur_pts[1][:, c0:TT], first_, last_, r=[ones_bf.b, cur_pts[1].b],
                   w=[Z2.b], inc=True)
            r1 = tmp()
            T.op("dve", lambda: nc.vector.reciprocal(out=r1[:, :], in_=Z1[:, :]), r=[Z1.b], w=[r1.b])
            t1 = tmp()
            T.op("dve", lambda: nc.vector.tensor_tensor(out=t1[:, :], in0=U1[:, :], in1=r1[:, :], op=ALU.mult),
                 r=[U1.b, r1.b], w=[t1.b])
            r2 = tmp()
            T.op("dve", lambda: nc.vector.reciprocal(out=r2[:, :], in_=Z2[:, :]), r=[Z2.b], w=[r2.b])
            t2 = tmp()
            T.op("dve", lambda: nc.vector.tensor_tensor(out=t2[:, :], in0=U2[:, :], in1=r2[:, :], op=ALU.mult),
                 r=[U2.b, r2.b], w=[t2.b])
            ot = ltmp()
            T.op("dve", lambda: nc.vector.scalar_tensor_tensor(
                out=ot[:, :], in0=t2[:, :], scalar=drv[:, 161:162], in1=t1[:, :], op0=ALU.mult, op1=ALU.add),
                r=[t1.b, t2.b, drv.b], w=[ot.b])
            T.op("act", lambda: nc.scalar.activation(out=osq[:, :], in_=ot[:, :], func=AF.Square), r=[ot.b], w=[osq.b])
            pz = ps(4, 8)
            st["ps"] += 1
            mm(pz[:, :], ones_bf[:, :], osq[:, :], True, True, r=[ones_bf.b, osq.b], w=[pz.b], inc=True)
            rs = ltmp()
            T.op("act", lambda: nc.scalar.activation(out=rs[:, :], in_=pz[:, :], func=AF.Sqrt, bias=RMS_EPS,
                                                     scale=1.0 / 128.0), r=[pz.b], w=[rs.b])
            T.op("dve", lambda: nc.vector.reciprocal(out=rs[:, :], in_=rs[:, :]), r=[rs.b], w=[rs.b])
            T.op("dve", lambda: nc.vector.tensor_tensor(out=ot[:, :], in0=ot[:, :], in1=rs[:, :], op=ALU.mult),
                 r=[ot.b, rs.b], w=[ot.b])
            T.op("act", lambda: nc.scalar.activation(out=hT[:, h, :], in_=ot[:, :], func=AF.Identity,
                                                     scale=drv[:, 166:167]), r=[ot.b, drv.b], w=[hT.b])
        if dbg == "attn_o" and qi == dbg_tile:
            dump(hT[:, :, :], hT.b, tile=qi)
        for sgi in range(4):
            s = load_slab(wb["wo"], 0, KC, sgi * 512)
            for j4 in range(4):
                mc = sgi * 4 + j4
                p = ps(4, 8)
                for kc in range(KC):
                    mm(p[:, :], s[:, kc, j4 * 128:(j4 + 1) * 128], hT[:, kc, :], kc == 0, kc == KC - 1,
                       r=[s.b, hT.b], w=[p.b], inc=(kc == KC - 1))
                G = MOD(1, 2)
                T.op("dve", lambda: nc.vector.scalar_tensor_tensor(
                    out=xs[:, mc, :], in0=p[:, :], scalar=G(mc), in1=xs[:, mc, :], op0=ALU.mult, op1=ALU.add),
                    r=[p.b, xs.b, modT.b], w=[xs.b])
        T.barrier()

    def moe_tile(qi):
        abuf = Buf("actD")
        actb = sbr("actD", [128, EFC, TT], BF16, 0, abuf)
        sq = sbr("sqD", [128, KC, TT], BF16, 0, abuf)
        cur["sq"] = sq
        combT = sbr("combT", [8, TT], F32, 57344)
        lgT = sbr("lgT", [8, TT], F32, 59392)
        sm = sbr("sm", [128, 256], F32, 61440)
        state = {}

        def post_chunk(c, t):
            if c == 0:
                state["pl"] = ps()
            pl = state["pl"]
            mm(pl[0:8, :], wr_sb[:, c, :], t[:, :], c == 0, c == KC - 1, r=[wr_sb.b, t.b], w=[pl.b], inc=True)

        if not front:
            T.dma("sp", xs[:, :, :], xview(x2T_in)[:, :, qi * TT:(qi + 1) * TT], w=[xs.b])
        rms_mod(DRV(96), MOD(1, 3), post_chunk)
        if (not front) and (not first):
            T.dma("sp", xs[:, :, :], xview(xacc_in)[:, :, qi * TT:(qi + 1) * TT], w=[xs.b])
        pl = state["pl"]
        T.op("act", lambda: nc.scalar.activation(out=lgT[0:8, :], in_=pl[0:8, :], func=AF.Copy), r=[pl.b], w=[lgT.b])
        for tb in range(4):
            o = tb * 64
            pt = ps()
            T.op("pe", lambda: nc.tensor.transpose(out=pt[:, 0:8], in_=lgT[0:8, tb * 128:(tb + 1) * 128],
                                                   identity=consts[0:8, C_ID:C_ID + 8]),
                 r=[lgT.b, consts.b], w=[pt.b])
            lg = sm[:, o:o + 8]
            eq1 = sm[:, o + 8:o + 16]
            l2 = sm[:, o + 16:o + 24]
            eq2 = sm[:, o + 24:o + 32]
            comb = sm[:, o + 32:o + 40]
            m1 = sm[:, o + 40:o + 41]
            m2 = sm[:, o + 41:o + 42]
            dlt = sm[:, o + 42:o + 43]
            w2 = sm[:, o + 43:o + 44]
            w1 = sm[:, o + 44:o + 45]
            sb_ = [sm.b]
            T.op("dve", lambda: nc.vector.tensor_copy(out=lg, in_=pt[:, 0:8]), r=[pt.b], w=sb_)
            T.op("dve", lambda: nc.vector.reduce_max(out=m1, in_=lg, axis=AX.X), r=sb_, w=sb_)
            T.op("dve", lambda: nc.vector.tensor_scalar(out=eq1, in0=lg, scalar1=m1, scalar2=None, op0=ALU.is_equal),
                 r=sb_, w=sb_)
            T.op("dve", lambda: nc.vector.scalar_tensor_tensor(out=l2, in0=eq1, scalar=-1e30, in1=lg, op0=ALU.mult,
                                                               op1=ALU.add), r=sb_, w=sb_)
            T.op("dve", lambda: nc.vector.reduce_max(out=m2, in_=l2, axis=AX.X), r=sb_, w=sb_)
            T.op("dve", lambda: nc.vector.tensor_scalar(out=eq2, in0=l2, scalar1=m2, scalar2=None, op0=ALU.is_equal),
                 r=sb_, w=sb_)
            T.op("dve", lambda: nc.vector.tensor_tensor(out=dlt, in0=m2, in1=m1, op=ALU.subtract), r=sb_, w=sb_)
            T.op("act", lambda: nc.scalar.activation(out=w2, in_=dlt, func=AF.Sigmoid), r=sb_, w=sb_)
            T.op("dve", lambda: nc.vector.tensor_scalar(out=w1, in0=w2, scalar1=-1.0, scalar2=1.0, op0=ALU.mult,
                                                        op1=ALU.add), r=sb_, w=sb_)
            T.op("dve", lambda: nc.vector.tensor_scalar(out=comb, in0=eq1, scalar1=w1, scalar2=None, op0=ALU.mult),
                 r=sb_, w=sb_)
            T.op("dve", lambda: nc.vector.scalar_tensor_tensor(out=comb, in0=eq2, scalar=w2, in1=comb, op0=ALU.mult,
                                                               op1=ALU.add), r=sb_, w=sb_)
            pt2 = ps()
            T.op("pe", lambda: nc.tensor.transpose(out=pt2[0:8, 0:128], in_=comb,
                                                   identity=consts[:, C_ID:C_ID + 128]),
                 r=[sm.b, consts.b], w=[pt2.b])
            T.op("act", lambda: nc.scalar.activation(out=combT[0:8, tb * 128:(tb + 1) * 128], in_=pt2[0:8, 0:128],
                                                     func=AF.Copy), r=[pt2.b], w=[combT.b])
        if dbg == "comb" and qi == dbg_tile:
            T.op("dve", lambda: nc.vector.memset(xs[:, 0:1, :], 0.0), w=[xs.b])
            T.op("act", lambda: nc.scalar.activation(out=xs[0:8, 0, :], in_=combT[0:8, :], func=AF.Copy),
                 r=[combT.b], w=[xs.b])
            dump(xs[:, :, :], xs.b, tile=qi)
        for e in range(ne_run):
            pcb = ps(4, 8)
            ge = e0 + e
            mm(pcb[:, :], consts[0:8, C_SEL + ge * 128:C_SEL + (ge + 1) * 128], combT[0:8, :], True, True,
               r=[consts.b, combT.b], w=[pcb.b], inc=True)
            cb = ltmp()
            T.op("act", lambda: nc.scalar.activation(out=cb[:, :], in_=pcb[:, :], func=AF.Copy), r=[pcb.b], w=[cb.b])
            gate_up(mwg_b[e], mwu_b[e], EF // 512, actb, cb=cb)
            down_proj(mwd_b[e], EFC, actb, MOD(1, 5), kgroup=14)
        if dbg == "moe1" and qi == dbg_tile:
            dump(xs[:, :, :], xs.b, tile=qi)
        if last:
            rstd = rms_stats(xs)
            for c in range(KC):
                T.op("dve", lambda: nc.vector.scalar_tensor_tensor(
                    out=xs[:, c, :], in0=xs[:, c, :], scalar=V(V_FING + c), in1=rstd[:, :], op0=ALU.mult,
                    op1=ALU.mult), r=[xs.b, rstd.b, vecs.b], w=[xs.b])
        T.dma("act", xview(outT.t)[:, :, qi * TT:(qi + 1) * TT], xs[:, :, :], r=[xs.b], w=[outT.b], accum=True)
        T.barrier()

    try:
        if front:
            phase_c1()
        for qi in range(NT):
            if dbg_tile is not None and qi != dbg_tile:
                continue
            if front:
                attention_tile(qi)
                if x2out:
                    T.dma("act", xview(x2T_out.t)[:, :, qi * TT:(qi + 1) * TT], xs[:, :, :], r=[xs.b],
                          w=[x2T_out.b], accum=True)
            if stage <= 2 or mode == "front":
                T.dma("act", xview(outT.t)[:, :, qi * TT:(qi + 1) * TT], xs[:, :, :], r=[xs.b], w=[outT.b],
                      accum=True)
                T.barrier()
            else:
                moe_tile(qi)
        if mode == "front" or x2out:
            T.dma("sp", mvec_out.t[:, 0:192], modT[:, :], r=[modT.b], w=[mvec_out.b], accum=True)
            T.dma("sp", mvec_out.t[:, 192:368], drv[:, :], r=[drv.b], w=[mvec_out.b], accum=True)
        T.barrier(full=True)
    except DbgStop:
        return nc
    return nc


def _pm(v):
    v = np.asarray(v, np.float32).reshape(-1, 128)
    return np.ascontiguousarray(v.T)


def _host_consts():
    consts = np.zeros((128, C_TOT), np.float32)
    consts[:, C_ID:C_ID + 128] = np.eye(128, dtype=np.float32)
    kk = np.arange(128)[:, None]
    qq = np.arange(128)[None, :]
    consts[:, C_ABSD:C_ABSD + 128] = np.abs(qq - kk).astype(np.float32)
    allowed = (kk // 64) <= (qq // 64)
    consts[:, C_DMASK:C_DMASK + 128] = np.where(allowed, 0.0, NEG).astype(np.float32)
    for e in range(NE):
        consts[e, C_SEL + e * 128:C_SEL + (e + 1) * 128] = 1.0
    base = (np.arange(S)[None, :] - np.arange(128)[:, None]).astype(np.float32)
    return consts, np.ascontiguousarray(base)


def make_in_maps(inputs, cores, stage=99, ne_run=NE):
    f = lambda k: np.asarray(inputs[k], np.float32)
    x = f("x")
    c = f("c")
    consts, base = _host_consts()
    shared = {
        "consts": consts, "base": base,
        "mod_w": np.ascontiguousarray(f("mod_w")),
        "cwin": np.ascontiguousarray(f("conv_w_in")[0]),
        "cwout": np.ascontiguousarray(f("conv_w_out")[0]),
        "fwg": np.ascontiguousarray(f("ffn_w_gate")[0]),
        "fwu": np.ascontiguousarray(f("ffn_w_up")[0]),
        "fwd": np.ascontiguousarray(f("ffn_w_down")[0]),
        "wqkv": np.ascontiguousarray(f("attn_w_qkv")[0]),
        "wo": np.ascontiguousarray(f("attn_w_o")[0]),
        "wr": np.ascontiguousarray(f("moe_w_router")[0]),
    }
    if stage >= 3:
        shared["mwg"] = np.ascontiguousarray(f("moe_w_gate")[0][:ne_run])
        shared["mwu"] = np.ascontiguousarray(f("moe_w_up")[0][:ne_run])
        shared["mwd"] = np.ascontiguousarray(f("moe_w_down")[0][:ne_run])
    vecs = np.zeros((128, V_TOT), np.float32)
    for l in range(2):
        vecs[:, V_N1G + l * 16:V_N1G + (l + 1) * 16] = _pm(f("norm1_g")[l])
        vecs[:, V_N2G + l * 16:V_N2G + (l + 1) * 16] = _pm(f("norm2_g")[l])
        vecs[:, V_MODB + l * 96:V_MODB + (l + 1) * 96] = _pm(f("mod_b")[l])
    vecs[:, V_CBIN:V_CBIN + 32] = _pm(f("conv_b_in")[0])
    wdw = f("conv_w_dw")[0]
    vecs[:, V_CWDW:V_CWDW + 16 * CW] = np.ascontiguousarray(
        wdw.reshape(CW, 16, 128).transpose(2, 1, 0)).reshape(128, 16 * CW)
    vecs[:, V_CBDW:V_CBDW + 16] = _pm(f("conv_b_dw")[0])
    vecs[:, V_LNG:V_LNG + 16] = _pm(f("conv_ln_g")[0])
    vecs[:, V_LNB:V_LNB + 16] = _pm(f("conv_ln_b")[0])
    vecs[:, V_CBOUT:V_CBOUT + 16] = _pm(f("conv_b_out")[0])
    vecs[:, V_FING:V_FING + 16] = _pm(f("final_g"))
    vecs[:, V_SUBG] = f("attn_subln_g")[0]
    lam = np.concatenate([f("attn_lam_q1")[0], f("attn_lam_k1")[0], f("attn_lam_q2")[0], f("attn_lam_k2")[0]])
    vecs[:, V_LAM:V_LAM + 256] = lam[None, :]
    maps = []
    for b in cores:
        m = dict(shared)
        m["vecs"] = vecs
        m["xT"] = np.ascontiguousarray(x[b].T).reshape(KC, 128, S)
        m["cT"] = _pm(c[b])
        maps.append(m)
    return maps


def _launch(nc, maps, trace=False):
    names = set()
    for alloc in nc.allocations:
        try:
            if alloc.kind == "ExternalInput":
                names.add(alloc.memorylocations[0].name)
        except Exception:
            pass
    maps = [{k: v for k, v in m.items() if k in names} for m in maps]
    return run_bass_kernel_spmd(nc, maps, core_ids=list(range(len(maps))), trace=trace)


def run(inputs, stage=99, cores=None, trace=False, dbg=None, dbg_tile=None, ne_run=NE):
    cores = list(range(B)) if cores is None else cores
    nc = build(stage=stage, dbg=dbg, dbg_tile=dbg_tile, ne_run=ne_run)
    maps = make_in_maps(inputs, cores, stage, ne_run)
    res = _launch(nc, maps, trace)
    outs = [np.ascontiguousarray(r["outT"].reshape(D, S).T) for r in res.results]
    return np.stack(outs, axis=0), res


NE_L1 = 4


def run_multi(inputs, cores=None, trace=False):
    cores = list(range(B)) if cores is None else cores
    maps = make_in_maps(inputs, cores, stage=3, ne_run=NE_L1)
    nc1 = build(stage=3, mode="full", ne_run=NE_L1, last=False, x2out=True)
    res = _launch(nc1, maps, trace)
    x2 = [r["x2T_out"] for r in res.results]
    acc = [r["outT"] for r in res.results]
    mvec = [r["mvec_out"] for r in res.results]
    f = lambda k: np.asarray(inputs[k], np.float32)
    ne2 = NE - NE_L1
    nc2 = build(stage=3, mode="moe", e0=0, ne_run=ne2, first=False, last=True)
    consts = maps[0]["consts"].copy()
    consts[:, C_SEL:] = 0.0
    for le in range(ne2):
        consts[NE_L1 + le, C_SEL + le * 128:C_SEL + (le + 1) * 128] = 1.0
    g = np.ascontiguousarray(f("moe_w_gate")[0][NE_L1:])
    u = np.ascontiguousarray(f("moe_w_up")[0][NE_L1:])
    d = np.ascontiguousarray(f("moe_w_down")[0][NE_L1:])
    mm_ = []
    for i in range(len(cores)):
        mm_.append({"vecs": maps[i]["vecs"], "consts": consts, "wr": maps[i]["wr"], "x2T": x2[i],
                    "xaccT": acc[i], "mvec": mvec[i], "mwg": g, "mwu": u, "mwd": d})
    res = _launch(nc2, mm_, trace)
    outs = [np.ascontiguousarray(r["outT"].reshape(D, S).T) for r in res.results]
    return np.stack(outs, axis=0)


def kernel(**inputs):
    out, _ = run(inputs)
    return out.astype(np.float32)
```
